# Optimizing a Trainium2 kernel written in Bass

```python
import math
import jax, jax.numpy as jnp
from jax import lax
import numpy as np

D_MODEL = 1024
BATCH = 4
SEQ = 4096
DEPTH = 1

CHUNK = 64
N_PREV_CHUNKS = 8
HEAD_DIM = 64
N_HEADS_A = 8
REL_CLIP = 128
N_HEADS_B = 8
N_KV_B = 2
N_IDX_HEADS = 8
IDX_DIM = 64
TOPK_MAX = 256
Q_BLOCK = 128
ROPE_THETA = 500000.0
ROT_DIM = HEAD_DIM // 4
D_FF = 2816
EPS = 1e-6

WIDTH_A = N_HEADS_A * HEAD_DIM
WIDTH_B = N_HEADS_B * HEAD_DIM
WIDTH_KV_B = N_KV_B * HEAD_DIM
SPLIT_SIZES = (WIDTH_A, WIDTH_A, WIDTH_A,
               WIDTH_B, WIDTH_KV_B, WIDTH_KV_B,
               N_IDX_HEADS * IDX_DIM, IDX_DIM,
               N_IDX_HEADS,
               D_MODEL, D_MODEL)
D_IN = sum(SPLIT_SIZES)

kernel_name = 'hybrid_chunked_relbias_dsa_macaron'


def _rmsnorm(x, g):
    xf = x.astype(jnp.float32)
    y = xf * lax.rsqrt(jnp.mean(xf * xf, axis=-1, keepdims=True) + EPS)
    return (y * g.astype(jnp.float32)).astype(x.dtype)


def _swiglu(x, w_in, w_out):
    gate, up = jnp.split(x @ w_in, 2, axis=-1)
    return (jax.nn.silu(gate) * up) @ w_out


def _rope_tables(seq):
    inv_freq = jnp.power(jnp.float32(ROPE_THETA), -jnp.arange(0, ROT_DIM, 2, dtype=jnp.float32) / ROT_DIM)
    ang = jnp.arange(seq, dtype=jnp.float32)[:, None] * inv_freq[None, :]
    return jnp.cos(ang)[:, None, :], jnp.sin(ang)[:, None, :]


def _partial_rope(t, cos, sin):
    tr = t[..., :ROT_DIM].astype(jnp.float32)
    t1, t2 = tr[..., :ROT_DIM // 2], tr[..., ROT_DIM // 2:]
    rot = jnp.concatenate([t1 * cos - t2 * sin, t2 * cos + t1 * sin], axis=-1)
    return jnp.concatenate([rot.astype(t.dtype), t[..., ROT_DIM:]], axis=-1)


def _mixer_a(q, k, v, rel_bias):
    B, S, H, Dh = q.shape
    nc = S // CHUNK
    band_len = (N_PREV_CHUNKS + 1) * CHUNK
    qc = q.reshape(B, nc, CHUNK, H, Dh)

    def band(t):
        tp = jnp.pad(t, ((0, 0), (N_PREV_CHUNKS * CHUNK, 0), (0, 0), (0, 0)))
        tp = tp.reshape(B, nc + N_PREV_CHUNKS, CHUNK, H, Dh)
        return jnp.concatenate([tp[:, j:j + nc] for j in range(N_PREV_CHUNKS + 1)], axis=2)

    kb, vb = band(k), band(v)
    s = jnp.einsum('bnqhd,bnkhd->bnhqk', qc, kb).astype(jnp.float32) * (Dh ** -0.5)
    qi = jnp.arange(CHUNK)
    kj = jnp.arange(band_len)
    dist = N_PREV_CHUNKS * CHUNK + qi[:, None] - kj[None, :]
    bias = rel_bias[:, jnp.clip(dist, -REL_CLIP, REL_CLIP) + REL_CLIP].astype(jnp.float32)
    key_pos = (jnp.arange(nc)[:, None] - N_PREV_CHUNKS) * CHUNK + kj[None, :]
    valid = (key_pos >= 0)[None, :, None, None, :]
    s = jnp.where(valid, s + bias[None, None], -jnp.inf)
    p = jax.nn.softmax(s, axis=-1).astype(vb.dtype)
    o = jnp.einsum('bnhqk,bnkhd->bnqhd', p, vb)
    return o.reshape(B, S, H * Dh)


def _to_blocks(a):
    B, S = a.shape[:2]
    return jnp.moveaxis(a.reshape((B, S // Q_BLOCK, Q_BLOCK) + a.shape[2:]), 1, 0)


def _mixer_b(q, k, v, q_idx, k_idx, w_idx):
    B, S, H, Dh = q.shape
    G = k.shape[2]
    R = H // G
    topk = min(TOPK_MAX, S // 4)
    nb = S // Q_BLOCK
    key_pos = jnp.arange(S)
    w_scaled = w_idx * (N_IDX_HEADS ** -0.5)

    def one_block(args):
        qb, qib, wb, pos_q = args
        sc = jnp.einsum('bqhd,bsd->bqhs', qib, k_idx).astype(jnp.float32) * (IDX_DIM ** -0.5)
        score = jnp.einsum('bqh,bqhs->bqs', wb.astype(jnp.float32), jax.nn.relu(sc))
        limit = (pos_q // CHUNK + 1) * CHUNK
        adm = key_pos[None, :] < limit[:, None]
        score = jnp.where(adm[None], score, -jnp.inf)
        _, idx = lax.top_k(score, topk)
        sel_ok = idx < limit[None, :, None]
        kg = jax.vmap(lambda t, i: t[i])(k, idx)
        vg = jax.vmap(lambda t, i: t[i])(v, idx)
        qg = qb.reshape(B, Q_BLOCK, G, R, Dh)
        s = jnp.einsum('bqgrd,bqkgd->bqgrk', qg, kg).astype(jnp.float32) * (Dh ** -0.5)
        s = jnp.where(sel_ok[:, :, None, None, :], s, -jnp.inf)
        p = jax.nn.softmax(s, axis=-1).astype(vg.dtype)
        o = jnp.einsum('bqgrk,bqkgd->bqgrd', p, vg)
        return o.reshape(B, Q_BLOCK, H * Dh)

    out = lax.map(one_block, (_to_blocks(q), _to_blocks(q_idx), _to_blocks(w_scaled),
                              key_pos.reshape(nb, Q_BLOCK)))
    return jnp.moveaxis(out, 0, 1).reshape(B, S, H * Dh)


def setup_inputs(seed: int = 0) -> dict:
    key = jax.random.key(seed)
    ks = jax.random.split(key, 16)
    f32 = jnp.float32

    def w(k, shape, fan_in):
        return jax.random.normal(k, shape, f32) * (fan_in ** -0.5)

    def gain(k):
        return 1.0 + 0.02 * jax.random.normal(k, (DEPTH, D_MODEL), f32)

    return {
        'x': jax.random.normal(ks[0], (BATCH, SEQ, D_MODEL), f32),
        'n1_g': gain(ks[1]),
        'ffn1_w_in': w(ks[2], (DEPTH, D_MODEL, 2 * D_FF), D_MODEL),
        'ffn1_w_out': w(ks[3], (DEPTH, D_FF, D_MODEL), D_FF),
        'n2_g': gain(ks[4]),
        'w_in': w(ks[5], (DEPTH, D_MODEL, D_IN), D_MODEL),
        'rel_bias': 0.2 * jax.random.normal(ks[6], (DEPTH, N_HEADS_A, 2 * REL_CLIP + 1), f32),
        'w_branch_a': w(ks[7], (DEPTH, WIDTH_A, D_MODEL), WIDTH_A),
        'w_branch_b': w(ks[8], (DEPTH, WIDTH_B, D_MODEL), WIDTH_B),
        'w_out': w(ks[9], (DEPTH, D_MODEL, D_MODEL), D_MODEL),
        'n3_g': gain(ks[10]),
        'ffn2_w_in': w(ks[11], (DEPTH, D_MODEL, 2 * D_FF), D_MODEL),
        'ffn2_w_out': w(ks[12], (DEPTH, D_FF, D_MODEL), D_FF),
        'nf_g': 1.0 + 0.02 * jax.random.normal(ks[13], (D_MODEL,), f32),
    }


def reference(x, n1_g, ffn1_w_in, ffn1_w_out, n2_g, w_in, rel_bias, w_branch_a, w_branch_b,
              w_out, n3_g, ffn2_w_in, ffn2_w_out, nf_g):
    B, S, _ = x.shape
    cos, sin = _rope_tables(S)
    split_points = list(np.cumsum(SPLIT_SIZES)[:-1])
    for l in range(DEPTH):
        x = x + 0.5 * _swiglu(_rmsnorm(x, n1_g[l]), ffn1_w_in[l], ffn1_w_out[l])
        h = _rmsnorm(x, n2_g[l])
        qa, ka, va, qb, kb, vb, qi, ki, wi, ga, gb = jnp.split(h @ w_in[l], split_points, axis=-1)
        qa = qa.reshape(B, S, N_HEADS_A, HEAD_DIM)
        ka = ka.reshape(B, S, N_HEADS_A, HEAD_DIM)
        va = va.reshape(B, S, N_HEADS_A, HEAD_DIM)
        qb = _partial_rope(qb.reshape(B, S, N_HEADS_B, HEAD_DIM), cos, sin)
        kb = _partial_rope(kb.reshape(B, S, N_KV_B, HEAD_DIM), cos, sin)
        vb = vb.reshape(B, S, N_KV_B, HEAD_DIM)
        qi = _partial_rope(qi.reshape(B, S, N_IDX_HEADS, IDX_DIM), cos, sin)
        ki = _partial_rope(ki.reshape(B, S, 1, IDX_DIM), cos, sin)[:, :, 0]
        o_a = _mixer_a(qa, ka, va, rel_bias[l])
        o_b = _mixer_b(qb, kb, vb, qi, ki, wi)
        merged = jax.nn.sigmoid(ga) * (o_a @ w_branch_a[l]) + jax.nn.sigmoid(gb) * (o_b @ w_branch_b[l])
        x = x + merged @ w_out[l]
        x = x + 0.5 * _swiglu(_rmsnorm(x, n3_g[l]), ffn2_w_in[l], ffn2_w_out[l])
    return _rmsnorm(x, nf_g)
```

```python
import contextlib
import numpy as np
import concourse.bass as bass
import concourse.mybir as mybir
from concourse.bass_utils import run_bass_kernel_spmd

F32 = mybir.dt.float32
BF16 = mybir.dt.bfloat16
AF = mybir.ActivationFunctionType
ALU = mybir.AluOpType

D = 1024
DFF = 2816
SEQ = 4096
NT = 8
TILE = 512
NIT = 16
NEG = -30000.0
NS = 5
NBLK = 80


class Res:
    __slots__ = ("name", "w", "r", "sem", "semcnt")

    def __init__(self, name):
        self.name = name
        self.w = None
        self.r = []
        self.sem = None
        self.semcnt = 0


class Prog:
    COMPUTE = ("pe", "act", "dve", "pool")

    def __init__(self, nc, stack):
        self.nc = nc
        self.stack = stack
        self.ops = {e: [] for e in ("pe", "act", "dve", "pool", "sp")}
        self.sems = {}
        self.cnt = {}
        for e in self.COMPUTE:
            self.sems[e] = stack.enter_context(nc.semaphore("s_" + e))
            self.cnt[e] = 0
        self.known = {e: {} for e in self.ops}
        self.ndsem = 0

    def res(self, name):
        return Res(name)

    def _res_sem(self, r):
        if r.sem is None:
            key = "d%d" % self.ndsem
            self.sems[key] = self.stack.enter_context(self.nc.semaphore("sd_%d" % self.ndsem))
            self.ndsem += 1
            r.sem = key
        return r.sem

    def _deps(self, eng, R, W, is_dma):
        waits = {}

        def need(ev, kind):
            if ev is None:
                return
            key, val, e2 = ev
            if (not is_dma) and e2 == eng and key == eng:
                if eng == "pe":
                    return
            if self.known[eng].get(key, 0) >= val:
                return
            if waits.get(key, 0) < val:
                waits[key] = val

        for r in R:
            need(r.w, "raw")
        for r in W:
            need(r.w, "waw")
            for ev in r.r:
                need(ev, "war")
        for k, v in waits.items():
            self.known[eng][k] = v
        return list(waits.items())

    def op(self, eng, fn, R=(), W=(), inc=True):
        waits = self._deps(eng, R, W, False)
        if inc:
            self.cnt[eng] += 1
            ev = (eng, self.cnt[eng], eng)
        else:
            ev = (eng, self.cnt[eng] + 1, eng)
        for r in R:
            r.r.append(ev)
        for r in W:
            r.w = ev
            r.r = []
        self.ops[eng].append((waits, fn, (eng, 1) if inc else None))

    def dma(self, q, out, in_, R=(), W=(), owner=None, **kw):
        if owner is None:
            owner = (list(W) + list(R))[0]
        waits = self._deps(q, R, W, True)
        key = self._res_sem(owner)
        owner.semcnt += 16
        ev = (key, owner.semcnt, "dma")
        for r in R:
            r.r.append(ev)
        for r in W:
            r.w = ev
            r.r = []
        self.ops[q].append((waits, lambda e: e.dma_start(out=out, in_=in_, **kw), (key, 16)))

    def final_wait(self, eng, resources):
        waits = {}
        for r in resources:
            evs = ([r.w] if r.w else []) + list(r.r)
            for key, val, _ in evs:
                if waits.get(key, 0) < val:
                    waits[key] = val
        self.ops[eng].append((list(waits.items()), None, None))

    def emit(self):
        nc = self.nc
        sems = self.sems

        def replay(name):
            def body(e):
                for waits, fn, inc in self.ops[name]:
                    for k, v in waits:
                        e.wait_ge(sems[k], v)
                    if fn is None:
                        continue
                    ins = fn(e)
                    if inc is not None:
                        ins.then_inc(sems[inc[0]], inc[1])
            return body

        with nc.Block() as block:
            block.sync(replay("sp"))
            block.tensor(replay("pe"))
            block.scalar(replay("act"))
            block.vector(replay("dve"))
            block.gpsimd(replay("pool"))


def build(n_tiles=NT, stop_after=None, use_scratch=True):
    nc = bass.Bass("TRN2", target_bir_lowering=False)

    def din(name, shape):
        return nc.dram_tensor(name, list(shape), F32, kind="ExternalInput").ap()

    x_d = din("x", [SEQ, D])
    cs_d = din("cs", [2, 128, SEQ])
    kb_d = din("kb", [128, 32])
    kbrow_d = din("kbrow", [128, 256])
    w1a_d = din("w1a", [D, 2 * DFF])
    w1b_d = din("w1b", [DFF, D])
    w2a_d = din("w2a", [D, 2 * DFF])
    w2b_d = din("w2b", [DFF, D])
    wk_d = din("wk", [D, 1536])
    wq_d = din("wq", [D, 5120])
    wab_d = din("wab", [D, D])
    wo_d = din("wo", [D, D])
    gcol_d = din("gcol", [128, 24])
    gf_d = din("gf", [1, D])
    rbg_d = din("rbg", [128, 8 * 5 * 128])
    amask_d = din("amask", [128, 5 * 128])
    emask_d = din("emask", [128, 8 * 128])
    ident_d = din("ident", [128, 128])
    cpw_d = din("cpw", [128, NIT + 1])
    y_d = nc.dram_tensor("y", [n_tiles * 256, D], F32, kind="ExternalOutput").ap()
    wscr_d = nc.dram_tensor("wscr", [NBLK, 128, 4096], BF16, kind="Internal").ap() if (use_scratch and n_tiles > 1) else None

    with contextlib.ExitStack() as st:
        P = Prog(nc, st)

        def sb(name, shape, dt):
            return st.enter_context(nc.sbuf_tensor(name, list(shape), dt))

        xt = sb("xt", [128, 4, D], F32)
        hTa = sb("hT", [128, 8, TILE], BF16)
        hTb = hTa
        big = sb("big", [128, 12288], BF16)
        aT = big[:, 0:11264].rearrange("p (a b) -> p a b", a=22)
        hb = aT[:, 0:8, :].rearrange("p a b -> p (a b)").rearrange("p (j f) -> p j f", j=4)
        sg = [sb("sg%d" % i, [128, TILE], F32) for i in range(2)]
        kAT = sb("kAT", [128, 4, 1024], BF16)
        vA = sb("vA", [128, 8, 8, 66], BF16)
        kBT = sb("kBT", [128, SEQ], BF16)
        vB = sb("vB", [128, 32, 2, 66], BF16)
        kiT = sb("kiT", [64, SEQ], BF16)
        qAT = [sb("qAT%d" % i, [128, 4, 256], BF16) for i in range(2)]
        qBT = [sb("qBT%d" % i, [128, 4, 256], BF16) for i in range(2)]
        qiT = sb("qiT", [64, 2, 8, 8, 16], BF16)
        wrepT = sb("wrepT", [128, 256], F32)
        score = big[:, 0:8192].bitcast(F32)
        mb = big[:, 8192:12288]
        PT = [sb("PT%d" % i, [128, 8, 128], BF16) for i in range(3)]
        jk = sb("jk", [128, 2 * SEQ], BF16)
        xn = jk[:, :].bitcast(F32).rearrange("p (b f) -> p b f", b=4)
        sqj = PT[0][:, :, :].rearrange("p a b -> p (a b)")
        rt = sg
        cst = sb("cst", [128, 2, TILE], F32)
        biasT = sb("biasT", [128, 8, 5, 128], BF16)
        emask = sb("emask_s", [128, 8, 128], BF16)
        ident = sb("ident_s", [128, 128], BF16)
        gfb = sb("gfb", [128, D], F32)
        gcol = sb("gcol_s", [128, 24], F32)
        kbt = sb("kbt", [128, 32], F32)
        kbrow = sb("kbrow_s", [128, 256], F32)
        cpw = sb("cpw_s", [128, NIT + 1], F32)
        oab = [sb("oab%d" % i, [128, 512], BF16) for i in range(2)]
        oT = sb("oT", [128, 8, 256], BF16)
        mT = sb("mT", [128, 8, 256], BF16)
        wsel = sb("wsel", [128, 8, 128], BF16)
        rl = [sb("rl%d" % i, [128, TILE], BF16) for i in range(2)]
        st_s = sb("stat", [128, 96], F32)
        wsl = [sb("wsl%d" % i, [128, 4096], BF16) for i in range(NS)]
        banks = [st.enter_context(nc.psum_tensor("bk%d" % i, [128, 512], F32)) for i in range(8)]

        R = {}

        def rs(name):
            if name not in R:
                R[name] = P.res(name)
            return R[name]

        Rbank = [P.res("bank%d" % i) for i in range(8)]
        Rwsl = [P.res("wsl%d" % i) for i in range(NS)]
        state = {"bank": 0, "wsl": 0, "alt": 0, "pt": 0, "rl": 0, "sg": 0, "rt": 0, "wblk": 0, "tile": 0}
        Rwst = [P.res("wst%d" % i) for i in range(NS)]
        Rwslh = [P.res("wslh%d" % i) for i in range(NS)]
        Rwready = [P.res("wready%d" % i) for i in range(NS)]

        def bank():
            i = state["bank"]
            state["bank"] = (i + 1) % 6
            return banks[i], Rbank[i]

        def wslot():
            i = state["wsl"]
            state["wsl"] = (i + 1) % NS
            return wsl[i], Rwsl[i]

        def rot(key, n):
            i = state[key]
            state[key] = (i + 1) % n
            return i

        pend_st = []

        def flush_stores():
            for (k_, flat, r, n_el) in pend_st:
                P.dma("sp", wscr_d[k_, :, 0:n_el], flat, R=[r], owner=Rwst[Rwsl.index(r)])
            del pend_st[:]

        def load_w(src2d, kc0, nkc, c0, ncols, slot=None, soff=0):
            if slot is None:
                slot = wslot()
            t, r = slot
            if pend_st and pend_st[0][2] is not r:
                flush_stores()
            n_el = nkc * ncols
            flat = t[:, soff:soff + n_el]
            dst = flat.rearrange("p (k c) -> p k c", k=nkc)
            k_ = state["wblk"]
            state["wblk"] += 1
            assert k_ < NBLK
            if wscr_d is not None and state["tile"] >= 1:
                P.dma("sp", flat, wscr_d[k_, :, 0:n_el], R=Rwready, W=[r], owner=Rwslh[Rwsl.index(r)])
            else:
                src = src2d[kc0 * 128:(kc0 + nkc) * 128, c0:c0 + ncols].rearrange("(k p) c -> p k c", p=128)
                P.dma("pool", dst, src, W=[r])
                if wscr_d is not None:
                    pend_st.append((k_, flat, r, n_el))
            return dst, r

        def mm(out, lhsT, rhs, start, stop, Rr, Ww, last, skip=False):
            P.op("pe", lambda e: e.matmul(out, lhsT, rhs, start=start, stop=stop, skip_group_check=skip), R=Rr, W=Ww, inc=last)

        def evac(out, in_, Rr, Ww, eng=None):
            if eng is None:
                eng = "act" if (state["alt"] % 2 == 0) else "dve"
                state["alt"] += 1
            if eng == "act":
                P.op("act", lambda e: e.activation(out=out, in_=in_, func=AF.Copy), R=Rr, W=Ww)
            else:
                P.op("dve", lambda e: e.tensor_copy(out, in_), R=Rr, W=Ww)

        P.dma("sp", xn, x_d[0:512, :].rearrange("(b p) f -> p b f", p=128),
              W=[rs("xn%d" % b) for b in range(4)] + [rs("jk"), rs("jka")], owner=rs("xnload"))
        P.dma("sp", cst[:, :, :], cs_d[:, :, 0:512].rearrange("t p n -> p t n"), W=[rs("cst")])
        Rc = rs("consts")
        P.dma("sp", gcol[:], gcol_d, W=[Rc])
        P.dma("sp", kbt[:], kb_d, W=[Rc])
        P.dma("sp", kbrow[:], kbrow_d, W=[Rc])
        P.dma("sp", cpw[:], cpw_d, W=[Rc])
        P.dma("sp", gfb[:], bass.AP(gf_d.tensor, 0, [[0, 128], [1, D]]), W=[Rc])
        Rc2 = rs("consts2")
        P.dma("pool", ident[:], ident_d, W=[Rc2])
        P.dma("pool", emask[:].rearrange("p a b -> p (a b)"), emask_d, W=[Rc2])
        Rsc = rs("score")
        Rbias = rs("biasT")
        P.dma("sp", score[:, 2560:3200], amask_d, W=[Rsc])
        for half in range(2):
            P.dma("sp", score[:, 0:2560], rbg_d[:, half * 2560:(half + 1) * 2560], W=[Rsc])
            tv = score[:, 0:2560].rearrange("p (h r) -> p h r", h=4)
            am = score[:, 2560:3200].unsqueeze(1).to_broadcast([128, 4, 640])
            P.op("dve", lambda e, tv=tv, am=am: e.tensor_tensor(out=tv, in0=tv, in1=am, op=ALU.add), R=[Rsc], W=[Rsc])
            bo = biasT[:, half * 4:(half + 1) * 4, :, :].rearrange("p h r q -> p h (r q)")
            P.op("dve", lambda e, tv=tv, bo=bo: e.tensor_scalar(out=bo, in0=tv, scalar1=8.0, scalar2=None, op0=ALU.mult),
                 R=[Rsc], W=[Rbias])
        Rva = [rs("vA%d" % j) for j in range(8)]
        RvB = [rs("vB%d" % j) for j in range(32)]
        for t_ in (qAT[0], qAT[1], qBT[0], qBT[1]):
            P.op("pool", lambda e, t_=t_: e.memset(t_[:, :, :], 0.0), W=[rs("qAT"), rs("qBT")])
        P.op("pool", lambda e: e.memset(vA[:, :, :, 64:66], 1.0), W=Rva)
        P.op("pool", lambda e: e.memset(vB[:, :, :, 64:66], 1.0), W=RvB)

        Rxt = [rs("xt%d" % b) for b in range(4)]
        Rxn = [rs("xn%d" % b) for b in range(4)]
        Rhb = [rs("aT")] * 4
        Rstat = rs("stat")
        RkAT = [rs("kAT%d" % j) for j in range(2)]
        RkB = [rs("kB%d" % j) for j in range(8)]
        Rki = [rs("ki%d" % j) for j in range(8)]

        def rmsnorm_to_hT(blocks, gi, hT, RhT, col0, from_xn=False):
            nb = len(blocks)
            xsrc = xn if from_xn else xt
            Rsrc = (lambda b: [Rxn[b], rs("jk"), rs("jka")]) if from_xn else (lambda b: [Rxt[b]])
            P.op("dve", lambda e: e.memset(st_s[:, 0:8], 0.0), W=[Rstat])
            for j, b in enumerate(blocks):
                P.op("act", lambda e, b=b, j=j: e.activation(out=sqj[:], in_=xsrc[:, b, :], func=AF.Square,
                                                             accum_out=st_s[:, j:j + 1]),
                     R=Rsrc(b) + [Rstat], W=[rs("PT0"), Rstat])
            P.op("dve", lambda e: e.tensor_scalar(out=st_s[:, 4:4 + nb], in0=st_s[:, 0:nb], scalar1=1.0 / D, scalar2=1e-6,
                                                  op0=ALU.mult, op1=ALU.add), R=[Rstat], W=[Rstat])
            P.op("act", lambda e: e.activation(out=st_s[:, 4:4 + nb], in_=st_s[:, 4:4 + nb], func=AF.Sqrt), R=[Rstat], W=[Rstat])
            P.op("dve", lambda e: e.reciprocal(st_s[:, 4:4 + nb], st_s[:, 4:4 + nb]), R=[Rstat], W=[Rstat])
            for j, b in enumerate(blocks):
                P.op("dve", lambda e, b=b, j=j: e.tensor_scalar(out=hb[:, j, :], in0=xsrc[:, b, :], scalar1=st_s[:, 4 + j:5 + j],
                                                               scalar2=None, op0=ALU.mult),
                     R=Rsrc(b) + [Rstat], W=[Rhb[j]])
            for kc in range(8):
                bk, rb = bank()
                for j in range(nb):
                    mm(bk[:, j * 128:(j + 1) * 128], hb[:, j, kc * 128:(kc + 1) * 128], ident[:], True, True,
                       [Rhb[j], Rc2], [rb], j == nb - 1)
                o = hT[:, kc, col0:col0 + nb * 128]
                i_ = bk[:, 0:nb * 128]
                gs = gcol[:, gi * 8 + kc:gi * 8 + kc + 1]
                if kc % 2 == 0:
                    P.op("dve", lambda e, o=o, i_=i_, gs=gs: e.tensor_scalar(out=o, in0=i_, scalar1=gs, scalar2=None, op0=ALU.mult),
                         R=[rb, Rc], W=[RhT])
                else:
                    P.op("act", lambda e, o=o, i_=i_, gs=gs: e.activation(out=o, in_=i_, func=AF.Identity, scale=gs),
                         R=[rb, Rc], W=[RhT])

        def ffn(hT, RhT, col0, ntok, blocks, wa_d, wb_d, from_xn=False):
            RaT = rs("aT")
            for g in range(11):
                slot = wslot()
                wg, rw = load_w(wa_d, 0, 8, g * 256, 256, slot=slot, soff=0)
                wu, _ = load_w(wa_d, 0, 8, DFF + g * 256, 256, slot=slot, soff=2048)
                for jj in range(2):
                    j = g * 2 + jj
                    bg, rbg_ = bank()
                    bu, rbu = bank()
                    for kc in range(8):
                        mm(bg[:, 0:ntok], wg[:, kc, jj * 128:(jj + 1) * 128], hT[:, kc, col0:col0 + ntok], kc == 0, kc == 7,
                           [rw, RhT], [rbg_], kc == 7)
                    for kc in range(8):
                        mm(bu[:, 0:ntok], wu[:, kc, jj * 128:(jj + 1) * 128], hT[:, kc, col0:col0 + ntok], kc == 0, kc == 7,
                           [rw, RhT], [rbu], kc == 7)
                    si = rot("sg", 2)
                    rsg = rs("sg%d" % si)
                    P.op("act", lambda e, si=si, bg=bg: e.activation(out=sg[si][:, 0:ntok], in_=bg[:, 0:ntok], func=AF.Silu),
                         R=[rbg_], W=[rsg])
                    P.op("dve", lambda e, si=si, bu=bu, j=j: e.tensor_tensor(out=aT[:, j, 0:ntok], in0=sg[si][:, 0:ntok],
                                                                             in1=bu[:, 0:ntok], op=ALU.mult),
                         R=[rsg, rbu], W=[RaT])
            for ch in range(2):
                pieces = []
                for (k0, nk) in ((0, 8), (8, 8), (16, 6)):
                    pieces.append(load_w(wb_d, k0, nk, ch * 512, 512))
                for j, b in enumerate(blocks):
                    bk, rb = bank()
                    for kc in range(22):
                        wp, rw = pieces[kc // 8]
                        mm(bk[:, :], aT[:, kc, j * 128:(j + 1) * 128], wp[:, kc % 8, :], kc == 0, kc == 21,
                           [RaT, rw], [rb], kc == 21)
                    xs = xt[:, b, ch * 512:(ch + 1) * 512]
                    xi = xn[:, b, ch * 512:(ch + 1) * 512] if from_xn else xs
                    Ri = [Rxn[b], rs("jk"), rs("jka")] if from_xn else [Rxt[b]]
                    P.op("dve", lambda e, xs=xs, xi=xi, bk=bk: e.scalar_tensor_tensor(out=xs, in0=bk[:, :], scalar=0.5, in1=xi,
                                                                                      op0=ALU.mult, op1=ALU.add),
                         R=[rb] + Ri, W=[Rxt[b]])

        def rope(out, bt, btp, np_, cols, c0, Rr, Ww, psplit=False):
            i1, i2 = 0, 1
            r1, r2 = rs("sg0"), rs("sg1")
            n = 1
            for s_ in bt.shape[1:]:
                n *= s_
            rep = n // cols
            if rep == 1:
                C = cst[0:np_, 0, c0:c0 + cols]
                S_ = cst[0:np_, 1, c0:c0 + cols]
                t1 = rt[i1][0:np_, 0:n]
                t2 = rt[i2][0:np_, 0:n]
            else:
                C = cst[0:np_, 0, c0:c0 + cols].unsqueeze(1).to_broadcast([np_, rep, cols])
                S_ = cst[0:np_, 1, c0:c0 + cols].unsqueeze(1).to_broadcast([np_, rep, cols])
                t1 = rt[i1][0:np_, 0:n].rearrange("p (a b) -> p a b", a=rep)
                t2 = rt[i2][0:np_, 0:n].rearrange("p (a b) -> p a b", a=rep)
            P.op("dve", lambda e: e.tensor_tensor(out=t1, in0=bt, in1=C, op=ALU.mult), R=Rr + [rs("cst")], W=[r1])
            P.op("dve", lambda e: e.tensor_tensor(out=t2, in0=btp, in1=S_, op=ALU.mult), R=Rr + [rs("cst")], W=[r2])
            if psplit:
                for (o_, p0_, p1_) in out:
                    P.op("dve", lambda e, o_=o_, p0_=p0_, p1_=p1_: e.tensor_tensor(out=o_, in0=t1[p0_:p1_], in1=t2[p0_:p1_], op=ALU.add),
                         R=[r1, r2], W=Ww)
            elif isinstance(out, list):
                for (o_, c_, n_, a_) in out:
                    a1 = rt[i1][0:np_, c_:c_ + n_].rearrange("p (a b) -> p a b", a=a_)
                    a2 = rt[i2][0:np_, c_:c_ + n_].rearrange("p (a b) -> p a b", a=a_)
                    P.op("dve", lambda e, o_=o_, a1=a1, a2=a2: e.tensor_tensor(out=o_, in0=a1, in1=a2, op=ALU.add), R=[r1, r2], W=Ww)
            else:
                P.op("dve", lambda e: e.tensor_tensor(out=out, in0=t1, in1=t2, op=ALU.add), R=[r1, r2], W=Ww)

        def kside(i, part):
            RhT = rs("hT")
            slot_j = i % 2
            if part == "early":
                yield from kside_early(i)
                return
            w0, rw0 = load_w(wk_d, 0, 8, 0, 512)
            for c in range(4):
                bk, rb = bank()
                for kc in range(8):
                    mm(bk[:, :], w0[:, kc, c * 128:(c + 1) * 128], hTb[:, kc, :], kc == 0, kc == 7, [rw0, RhT], [rb], kc == 7)
                evac(kAT[:, c, slot_j * 512:(slot_j + 1) * 512], bk[:, :], [rb], [RkAT[slot_j]])
            yield
            w1, rw1 = load_w(wk_d, 0, 8, 512, 512)
            for b in range(4):
                bk, rb = bank()
                for kc in range(8):
                    mm(bk[:, :], hTb[:, kc, b * 128:(b + 1) * 128], w1[:, kc, :], kc == 0, kc == 7, [RhT, rw1], [rb], kc == 7)
                rt_ = (4 * i + b) % 8
                evac(vA[:, rt_, :, 0:64], bk[:, :].rearrange("p (h d) -> p h d", h=8), [rb], [Rva[rt_]])
                if b % 2 == 1:
                    yield

        def kside_early(i):
            RhT = rs("hT")
            w2, rw2 = load_w(wk_d, 0, 8, 1024, 512)
            b1, rb1 = bank()
            b2, rb2 = bank()
            for kc in range(8):
                mm(b1[:, :], w2[:, kc, 0:128], hTb[:, kc, :], kc == 0, kc == 7, [rw2, RhT], [rb1], kc == 7)
            for kc in range(8):
                mm(b2[:, :], w2[:, kc, 128:256], hTb[:, kc, :], kc == 0, kc == 7, [rw2, RhT], [rb2], kc == 7)
            rope(kBT[:, i * 512:(i + 1) * 512], b1[:, :], b2[:, :], 128, 512, 0, [rb1, rb2], [RkB[i]])
            b3, rb3 = bank()
            for b in range(4):
                for kc in range(8):
                    mm(b3[:, b * 128:(b + 1) * 128], hTb[:, kc, b * 128:(b + 1) * 128], w2[:, kc, 256:384], kc == 0, kc == 7,
                       [RhT, rw2], [rb3], (kc == 7 and b == 3))
            evac(vB[:, 4 * i:4 * i + 4, :, 0:64], b3[:, :].rearrange("p (b g d) -> p b g d", b=4, g=2), [rb3],
                 [RvB[4 * i + b] for b in range(4)])
            b4, rb4 = bank()
            b5, rb5 = bank()
            for kc in range(8):
                mm(b4[0:64, :], w2[:, kc, 384:448], hTb[:, kc, :], kc == 0, kc == 7, [rw2, RhT], [rb4], kc == 7)
            for kc in range(8):
                mm(b5[0:64, :], w2[:, kc, 448:512], hTb[:, kc, :], kc == 0, kc == 7, [rw2, RhT], [rb5], kc == 7)
            rope(kiT[0:64, i * 512:(i + 1) * 512], b4[0:64, :], b5[0:64, :], 64, 512, 0, [rb4, rb5], [Rki[i]])
            yield

        def qside(part):
            RhT = rs("hT")
            h2o = lambda kc: hTb[:, kc, 256:512]
            if part == "early":
                yield from qside_early()
                return
            w0, rw0 = load_w(wq_d, 0, 8, 0, 512)
            for c in range(4):
                bk, rb = bank()
                for kc in range(8):
                    mm(bk[:, 0:256], w0[:, kc, c * 128:(c + 1) * 128], h2o(kc), kc == 0, kc == 7, [rw0, RhT], [rb], kc == 7)
                evac(qAT[0][0:64, c, :], bk[0:64, 0:256], [rb], [rs("qAT")])
                evac(qAT[1][64:128, c, :], bk[64:128, 0:256], [rb], [rs("qAT")])
            yield
            w1, rw1 = load_w(wq_d, 0, 8, 512, 512)
            w2, rw2 = load_w(wq_d, 0, 8, 1024, 512)
            for cp in range(2):
                bt, rbt = bank()
                bp, rbp = bank()
                for cc in range(2):
                    c = cp * 2 + cc
                    for kc in range(8):
                        mm(bt[:, cc * 256:(cc + 1) * 256], w1[:, kc, c * 128:(c + 1) * 128], h2o(kc), kc == 0, kc == 7,
                           [rw1, RhT], [rbt], (kc == 7 and cc == 1))
                for cc in range(2):
                    c = cp * 2 + cc
                    for kc in range(8):
                        mm(bp[:, cc * 256:(cc + 1) * 256], w2[:, kc, c * 128:(c + 1) * 128], h2o(kc), kc == 0, kc == 7,
                           [rw2, RhT], [rbp], (kc == 7 and cc == 1))
                outs = [(qBT[0][0:64, cp * 2:cp * 2 + 2, :], 0, 64), (qBT[1][64:128, cp * 2:cp * 2 + 2, :], 64, 128)]
                rope(outs, bt[:, :].rearrange("p (a b) -> p a b", a=2),
                     bp[:, :].rearrange("p (a b) -> p a b", a=2), 128, 256, 256, [rbt, rbp], [rs("qBT")], psplit=True)
                yield

        def qside_early():
            RhT = rs("hT")
            h2o = lambda kc: hTb[:, kc, 256:512]
            w3, rw3 = load_w(wq_d, 0, 8, 1536, 512)
            w4, rw4 = load_w(wq_d, 0, 8, 2048, 512)
            for hp in range(4):
                bt, rbt = bank()
                bp, rbp = bank()
                for hh in range(2):
                    h = hp * 2 + hh
                    for kc in range(8):
                        mm(bt[0:64, hh * 256:(hh + 1) * 256], w3[:, kc, h * 64:(h + 1) * 64], h2o(kc), kc == 0, kc == 7,
                           [rw3, RhT], [rbt], (kc == 7 and hh == 1))
                for hh in range(2):
                    h = hp * 2 + hh
                    for kc in range(8):
                        mm(bp[0:64, hh * 256:(hh + 1) * 256], w4[:, kc, h * 64:(h + 1) * 64], h2o(kc), kc == 0, kc == 7,
                           [rw4, RhT], [rbp], (kc == 7 and hh == 1))
                outs = [(qiT[0:64, bl, :, hp * 2 + hh, :], hh * 256 + bl * 128, 128, 8) for hh in range(2) for bl in range(2)]
                rope(outs, bt[0:64, :].rearrange("p (a b) -> p a b", a=2),
                     bp[0:64, :].rearrange("p (a b) -> p a b", a=2), 64, 256, 256, [rbt, rbp], [rs("qiT")])
            w5, rw5 = load_w(wq_d, 0, 8, 2560, 128)
            bk, rb = bank()
            for kc in range(8):
                mm(bk[:, 0:256], w5[:, kc, :], h2o(kc), kc == 0, kc == 7, [rw5, RhT], [rb], kc == 7)
            evac(wrepT[:, :], bk[:, 0:256], [rb], [rs("wrepT")])
            yield

        def pv_finish(acc_banks, racc, osb, ro, kc0, q0):
            for g in range(2):
                accv = banks[6 + g][:, 0:264].rearrange("p (h d) -> p h d", h=4)
                rec = st_s[:, 16 + 4 * g:20 + 4 * g]
                P.op("dve", lambda e, accv=accv, rec=rec: e.reciprocal(rec, accv[:, :, 64]), R=[racc[g]], W=[Rstat])
                ov = osb[:, g * 256:(g + 1) * 256].rearrange("p (h d) -> p h d", h=4)
                rb_ = rec.unsqueeze(2).to_broadcast([128, 4, 64])
                P.op("dve", lambda e, ov=ov, accv=accv, rb_=rb_: e.tensor_tensor(out=ov, in0=accv[:, :, 0:64], in1=rb_, op=ALU.mult),
                     R=[racc[g], Rstat], W=[ro])
            bk, rb = bank()
            for c in range(4):
                mm(bk[:, c * 128:(c + 1) * 128], osb[:, c * 128:(c + 1) * 128], ident[:], True, True, [ro, Rc2], [rb], c == 3)
            evac(oT[:, kc0:kc0 + 4, q0:q0 + 128], bk[:, :].rearrange("p (c q) -> p c q", c=4), [rb], [rs("oT")])

        Racc = [Rbank[6], Rbank[7]]

        def mixer_a(i, ob):
            q0 = (ob - 2) * 128
            Tq = 4 * i + ob
            tiles = [T for T in range(Tq - 4, Tq + 1) if T >= 0]
            pend = None
            for idx, T in enumerate(tiles):
                rel = T - (Tq - 4)
                col = (T % 8) * 128
                jslot = (T // 4) % 2
                pi = rot("pt", 3)
                rpt = rs("PT%d" % pi)
                for g in range(2):
                    bk, rb = bank()
                    mm(bk[:, :].rearrange("p (h q) -> p h q", h=4), ident[:], biasT[:, g * 4:(g + 1) * 4, rel, :],
                       True, False, [Rc2, Rbias], [rb], False)
                    for hh in range(4):
                        h = g * 4 + hh
                        c, po = h // 2, (h % 2) * 64
                        mm(bk[:, hh * 128:(hh + 1) * 128], kAT[:, c, col:col + 128], qAT[h % 2][:, c, q0:q0 + 128],
                           False, hh == 3, [RkAT[jslot], rs("qAT")], [rb], hh == 3)
                    P.op("act", lambda e, bk=bk, pi=pi, g=g, T=T: e.activation(
                        out=PT[pi][:, g * 4:(g + 1) * 4, :], in_=bk[:, :].rearrange("p (h q) -> p h q", h=4),
                        func=AF.Exp, scale=0.125, bias=kbt[:, T:T + 1]), R=[rb, Rc], W=[rpt])
                if pend is not None:
                    pend()

                def do_pv(T=T, idx=idx, pi=pi, rpt=rpt):
                    for h in range(8):
                        mm(banks[6 + h // 4][:, (h % 4) * 66:(h % 4) * 66 + 66], PT[pi][:, h, :], vA[:, T % 8, h, :],
                           idx == 0 and h % 4 == 0, idx == len(tiles) - 1, [rpt, Rva[T % 8]], [Racc[h // 4]], h % 4 == 3, skip=True)
                pend = do_pv
                yield
            pend()
            pv_finish(None, Racc, oab[0], rs("oab0"), 0, q0)
            yield

        def idx_scores(i, ob):
            q0 = (ob - 2) * 128
            S = 512 * i + 128 * (ob + 1)
            Rw = rs("wsel")
            wb = wrepT[:, q0:q0 + 128].unsqueeze(1).to_broadcast([128, 8, 128])
            P.op("dve", lambda e: e.tensor_tensor(out=wsel[:, :, :], in0=emask[:, :, :], in1=wb, op=ALU.mult),
                 R=[Rc2, rs("wrepT")], W=[Rw])
            cscale = 1.0 / (8.0 * (8.0 ** 0.5))
            for ks in range(0, S, 512):
                n = min(512, S - ks)
                kres = [Rki[ks // 512]]
                ai = 6 + (ks // 512) % 2
                acc, racc = banks[ai], Rbank[ai]
                pend = None
                for g in range(8):
                    bs, rbs = bank()
                    mm(bs[:, 0:n], qiT[0:64, ob - 2, g, :, :].rearrange("p h q -> p (h q)"), kiT[0:64, ks:ks + n], True, True,
                       [rs("qiT")] + kres, [rbs], True)
                    ri = rot("rl", 2)
                    rrl = rs("rl%d" % ri)
                    P.op("act", lambda e, ri=ri, bs=bs, n=n: e.activation(out=rl[ri][:, 0:n], in_=bs[:, 0:n], func=AF.Relu, scale=cscale),
                         R=[rbs], W=[rrl])
                    if pend is not None:
                        pend()

                    def do_acc(g=g, ri=ri, rrl=rrl, n=n):
                        mm(acc[:, 0:n], wsel[:, g, :], rl[ri][:, 0:n], g == 0, g == 7, [Rw, rrl], [racc], g == 7)
                    pend = do_acc
                pend()
                evac(score[:, ks:ks + n], acc[:, 0:n], [racc], [Rsc])
            return S

        def bisect_gen(S):
            Rb = rs("bstat")
            Rcn = rs("bcnt")
            Rsg = rs("bsgn")
            Rjk = rs("jk")
            Rjka = rs("jka")
            split = S >= 1024
            S1 = ((S // 128) // 2) * 128 if split else S
            n2 = S - S1
            P.op("dve", lambda e: e.tensor_tensor(out=score[:, 0:256], in0=score[:, 0:256], in1=kbrow[:, :], op=ALU.add),
                 R=[Rsc, Rc], W=[Rsc])
            P.op("dve", lambda e: e.memset(score[0:64, S - 64:S], -200.0), W=[Rsc])
            sv = score[:, 0:S]
            jv = jk[:, 0:S]
            m8 = st_s[:, 24:32]
            lo0 = st_s[:, 32:33]
            w0 = st_s[:, 33:34]
            cnt = st_s[:, 34:35]
            dd = st_s[:, 35:36]
            mids = [st_s[:, 36:37], st_s[:, 37:38]]
            sgn = st_s[:, 39:40]
            w0c = st_s[:, 40:40 + NIT + 1]
            P.op("dve", lambda e: e.max(out=m8, in_=sv), R=[Rsc], W=[Rb])
            P.op("dve", lambda e: e.tensor_scalar(out=jv, in0=sv, scalar1=-100.0, scalar2=None, op0=ALU.max, op1=ALU.min,
                                                  accum_out=lo0), R=[Rsc, Rb], W=[Rjk, Rjka, Rb])
            P.op("dve", lambda e: e.tensor_tensor(out=w0, in0=m8[:, 0:1], in1=lo0, op=ALU.subtract), R=[Rb], W=[Rb])
            P.op("dve", lambda e: e.tensor_scalar(out=w0, in0=w0, scalar1=1.001, scalar2=1e-4, op0=ALU.mult, op1=ALU.add),
                 R=[Rb], W=[Rb])
            P.op("dve", lambda e: e.tensor_scalar(out=w0c, in0=cpw[:, :], scalar1=w0, scalar2=None, op0=ALU.mult),
                 R=[Rb, Rc], W=[Rb])
            P.op("dve", lambda e: e.tensor_tensor(out=mids[0], in0=lo0, in1=w0c[:, 0:1], op=ALU.add), R=[Rb], W=[Rb])
            yield
            for n in range(NIT):
                mcur, mnext = mids[n % 2], mids[(n + 1) % 2]
                P.op("dve", lambda e, mcur=mcur: e.tensor_scalar(out=jk[:, 0:S1], in0=score[:, 0:S1], scalar1=mcur, scalar2=None,
                                                                 op0=ALU.is_ge, op1=ALU.add, accum_out=cnt),
                     R=[Rsc, Rb], W=[Rjk, Rcn])
                if split:
                    P.op("act", lambda e, mcur=mcur: e.activation(out=jk[:, S1:S], in_=score[:, S1:S], func=AF.Sign, scale=-1.0,
                                                                 bias=mcur, accum_out=sgn), R=[Rsc, Rb], W=[Rjka, Rsg])
                    P.op("dve", lambda e: e.scalar_tensor_tensor(out=cnt, in0=sgn, scalar=-0.5, in1=cnt, op0=ALU.mult, op1=ALU.add),
                         R=[Rsg, Rcn], W=[Rcn])
                tval = 255.5 - n2 / 2.0
                P.op("dve", lambda e, tval=tval: e.tensor_scalar(out=dd, in0=cnt, scalar1=tval, scalar2=0.5, op0=ALU.is_ge,
                                                                 op1=ALU.subtract), R=[Rcn], W=[rs("bdd")])
                P.op("dve", lambda e, mcur=mcur, mnext=mnext, n=n: e.scalar_tensor_tensor(
                    out=mnext, in0=dd, scalar=w0c[:, n:n + 1], in1=mcur, op0=ALU.mult, op1=ALU.add), R=[rs("bdd"), Rb], W=[Rb])
                yield

        def bisect_final(S):
            Rb = rs("bstat")
            thr = st_s[:, 38:39]
            mfin = st_s[:, 36 + NIT % 2:37 + NIT % 2]
            w0c = st_s[:, 40:40 + NIT + 1]
            P.op("dve", lambda e: e.tensor_tensor(out=thr, in0=mfin, in1=w0c[:, NIT:NIT + 1], op=ALU.subtract), R=[Rb], W=[Rb])
            P.op("dve", lambda e: e.tensor_scalar(out=mb[:, 0:S], in0=score[:, 0:S], scalar1=thr, scalar2=8.0 * NEG, op0=ALU.is_lt,
                                                  op1=ALU.mult), R=[Rsc, Rb], W=[rs("mb")])

        def run_interleaved(gens):
            gens = list(gens)
            while gens:
                for g in list(gens):
                    try:
                        next(g)
                    except StopIteration:
                        gens.remove(g)

        def chain(*gs):
            for g in gs:
                yield from g

        def mixer_b(i, ob):
            q0 = (ob - 2) * 128
            S = 512 * i + 128 * (ob + 1)
            nT = S // 128
            Rmb = rs("mb")
            idb = ident[:, :].unsqueeze(1).to_broadcast([128, 4, 128])
            pend = None
            for T in range(nT):
                pi = rot("pt", 3)
                rpt = rs("PT%d" % pi)
                for g in range(2):
                    bk, rb = bank()
                    bv = bk[:, :].rearrange("p (h q) -> p h q", h=4)
                    mm(bv, kBT[:, T * 128:(T + 1) * 128], qBT[g][:, :, q0:q0 + 128], True, False,
                       [RkB[T // 4], rs("qBT")], [rb], False)
                    mm(bv, mb[:, T * 128:(T + 1) * 128], idb, False, True, [Rmb, Rc2], [rb], True)
                    P.op("act", lambda e, bv=bv, pi=pi, g=g: e.activation(out=PT[pi][:, g * 4:(g + 1) * 4, :], in_=bv,
                                                                          func=AF.Exp, scale=0.125), R=[rb], W=[rpt])
                if pend is not None:
                    pend()

                def do_pv(T=T, pi=pi, rpt=rpt):
                    for h in range(8):
                        mm(banks[6 + h // 4][:, (h % 4) * 66:(h % 4) * 66 + 66], PT[pi][:, h, :], vB[:, T, h // 4, :],
                           T == 0 and h % 4 == 0, T == nT - 1, [rpt, RvB[T]], [Racc[h // 4]], h % 4 == 3, skip=True)
                pend = do_pv
                yield
            pend()
            pv_finish(None, Racc, oab[1], rs("oab1"), 4, q0)
            yield

        sgab = xt[:, 0:2, :].rearrange("p a b -> p (a b)").bitcast(BF16).rearrange("p (c t) -> p c t", c=16)

        def gates_gen():
            RhT = rs("hT")
            for half in range(2):
                wga, rwga = load_w(wq_d, 0, 8, 3072 + half * 512, 512)
                wgb, rwgb = load_w(wq_d, 0, 8, 4096 + half * 512, 512)
                for cc in range(4):
                    c = half * 4 + cc
                    bg, rbg_ = bank()
                    for kc in range(8):
                        mm(bg[:, 0:256], wga[:, kc, cc * 128:(cc + 1) * 128], hTb[:, kc, 256:512], kc == 0, kc == 7,
                           [rwga, RhT], [rbg_], False)
                    for kc in range(8):
                        mm(bg[:, 256:512], wgb[:, kc, cc * 128:(cc + 1) * 128], hTb[:, kc, 256:512], kc == 0, kc == 7,
                           [rwgb, RhT], [rbg_], kc == 7)
                    P.op("act", lambda e, c=c, bg=bg: e.activation(out=sgab[:, 2 * c:2 * c + 2, :],
                                                                   in_=bg[:, :].rearrange("p (a t) -> p a t", a=2), func=AF.Sigmoid),
                         R=[rbg_], W=[Rxt[0], Rxt[1]])
                    yield

        def merge_and_out():
            RhT = rs("hT")
            RmT = rs("mT")
            for half in range(2):
                wab, rwab = load_w(wab_d, 0, 8, half * 512, 512)
                for cc in range(4):
                    c = half * 4 + cc
                    bp, rbp = bank()
                    for kc in range(4):
                        mm(bp[:, 0:256], wab[:, kc, cc * 128:(cc + 1) * 128], oT[:, kc, :], kc == 0, kc == 3,
                           [rwab, rs("oT")], [rbp], False)
                    for kc in range(4, 8):
                        mm(bp[:, 256:512], wab[:, kc, cc * 128:(cc + 1) * 128], oT[:, kc, :], kc == 4, kc == 7,
                           [rwab, rs("oT")], [rbp], kc == 7)
                    si = rot("sg", 2)
                    rsg = rs("sg%d" % si)
                    gv = sgab[:, 2 * c:2 * c + 2, :].rearrange("p a t -> p (a t)")
                    P.op("dve", lambda e, si=si, bp=bp, gv=gv: e.tensor_tensor(out=sg[si][:, :], in0=gv, in1=bp[:, :], op=ALU.mult),
                         R=[Rxt[0], Rxt[1], rbp], W=[rsg])
                    P.op("dve", lambda e, si=si, c=c: e.tensor_tensor(out=mT[:, c, :], in0=sg[si][:, 0:256], in1=sg[si][:, 256:512],
                                                                       op=ALU.add), R=[rsg], W=[RmT])
            for ch in range(2):
                wo, rwo = load_w(wo_d, 0, 8, ch * 512, 512)
                for j in range(2):
                    b = 2 + j
                    bk, rb = bank()
                    for kc in range(8):
                        mm(bk[:, :], mT[:, kc, j * 128:(j + 1) * 128], wo[:, kc, :], kc == 0, kc == 7, [RmT, rwo], [rb], kc == 7)
                    xs = xt[:, b, ch * 512:(ch + 1) * 512]
                    P.op("dve", lambda e, xs=xs, bk=bk: e.tensor_tensor(out=xs, in0=bk[:, :], in1=xs, op=ALU.add),
                         R=[rb, Rxt[b]], W=[Rxt[b]])

        def final_out(i):
            P.op("dve", lambda e: e.memset(st_s[:, 0:8], 0.0), W=[Rstat])
            for j in range(2):
                b = 2 + j
                P.op("act", lambda e, b=b, j=j: e.activation(out=sqj[:], in_=xt[:, b, :], func=AF.Square, accum_out=st_s[:, j:j + 1]),
                     R=[Rxt[b], Rstat], W=[rs("PT0"), Rstat])
            P.op("dve", lambda e: e.tensor_scalar(out=st_s[:, 4:6], in0=st_s[:, 0:2], scalar1=1.0 / D, scalar2=1e-6,
                                                  op0=ALU.mult, op1=ALU.add), R=[Rstat], W=[Rstat])
            P.op("act", lambda e: e.activation(out=st_s[:, 4:6], in_=st_s[:, 4:6], func=AF.Sqrt), R=[Rstat], W=[Rstat])
            P.op("dve", lambda e: e.reciprocal(st_s[:, 4:6], st_s[:, 4:6]), R=[Rstat], W=[Rstat])
            for j in range(2):
                b = 2 + j
                P.op("dve", lambda e, b=b, j=j: e.scalar_tensor_tensor(out=xt[:, b, :], in0=xt[:, b, :], scalar=st_s[:, 4 + j:5 + j],
                                                                       in1=gfb[:, :], op0=ALU.mult, op1=ALU.mult),
                     R=[Rxt[b], Rstat, Rc], W=[Rxt[b]])
                P.dma("pool", y_d[i * 256 + j * 128:i * 256 + (j + 1) * 128, :], xt[:, b, :], R=[Rxt[b]], owner=rs("yout%d" % j))

        def attn(i):
            S2 = idx_scores(i, 2)
            run_interleaved([bisect_gen(S2), chain(kside(i, "rest"), qside("rest"), mixer_a(i, 2), mixer_a(i, 3), gates_gen())])
            bisect_final(S2)
            S3 = idx_scores(i, 3)
            run_interleaved([bisect_gen(S3), mixer_b(i, 2)])
            bisect_final(S3)
            run_interleaved([mixer_b(i, 3)])

        def prefetch(i):
            P.dma("sp", xn, x_d[i * 512:(i + 1) * 512, :].rearrange("(b p) f -> p b f", p=128), W=Rxn + [rs("jk"), rs("jka")],
                  owner=rs("xnload"))
            P.dma("sp", cst[:, :, :], cs_d[:, :, i * 512:(i + 1) * 512].rearrange("t p n -> p t n"), W=[rs("cst")])

        def dump(i):
            for j in range(2):
                b = 2 + j
                P.dma("sp", y_d[i * 256 + j * 128:i * 256 + (j + 1) * 128, :], xt[:, b, :], R=[Rxt[b]], owner=rs("yout%d" % j))

        for i in range(n_tiles):
            state["tile"] = i
            state["wblk"] = 0
            if i == 1 and wscr_d is not None:
                flush_stores()
                for a_, b_ in zip(Rwready, Rwst):
                    a_.w = (b_.sem, b_.semcnt, "dma")
            stages = [
                ("load", lambda: None),
                ("norm1", lambda: rmsnorm_to_hT([0, 1, 2, 3], 0, hTa, rs("hT"), 0, from_xn=True)),
                ("ffn1", lambda: ffn(hTa, rs("hT"), 0, 512, [0, 1, 2, 3], w1a_d, w1b_d, from_xn=True)),
                ("norm2", lambda: rmsnorm_to_hT([0, 1, 2, 3], 1, hTb, rs("hT"), 0)),
                ("early", lambda: run_interleaved([chain(kside(i, "early"), qside("early"))])),
                ("attn", lambda: (attn(i), prefetch(i + 1) if i + 1 < n_tiles else None)),
                ("merge", lambda: merge_and_out()),
                ("norm3", lambda: rmsnorm_to_hT([2, 3], 2, hTa, rs("hT"), 0)),
                ("ffn2", lambda: ffn(hTa, rs("hT"), 0, 256, [2, 3], w2a_d, w2b_d)),
                ("final", lambda: final_out(i)),
            ]
            done = False
            for name, fn in stages:
                fn()
                if name == stop_after:
                    dump(i)
                    done = True
                    break
            if done:
                break
        P.final_wait("pool", [rs("yout0"), rs("yout1")])
        P.emit()
    return nc


SPLIT = (512, 512, 512, 512, 128, 128, 512, 64, 8, 1024, 1024)


def _rope_perm(ncols):
    p = np.arange(ncols)
    d = p % 64
    q = p.copy()
    q[d < 8] = p[d < 8] + 8
    m = (d >= 8) & (d < 16)
    q[m] = p[m] - 8
    return q


def _host_consts():
    c = {}
    c["ident"] = np.eye(128, dtype=np.float32)
    p = np.arange(128)
    em = np.zeros((128, 8, 128), np.float32)
    for g in range(8):
        em[p, g, 16 * g + (p % 16)] = 1.0
    c["emask"] = em.reshape(128, 1024)
    n = np.arange(NIT + 1, dtype=np.float64)
    c["cpw"] = np.broadcast_to((2.0 ** -(n + 1)).astype(np.float32), (128, NIT + 1)).copy()
    k = np.arange(128)[:, None, None]
    rel = np.arange(5)[None, :, None]
    q = np.arange(128)[None, None, :]
    kc_rel = 2 * (rel - 4) + k // 64
    cq = q // 64
    ok = (kc_rel >= cq - 8) & (kc_rel <= cq)
    c["amask"] = np.where(ok, 0.0, NEG).astype(np.float32).reshape(128, 640)
    dist = np.clip(q - k + 512 - 128 * rel, -128, 128) + 128
    c["_dist"] = np.broadcast_to(dist, (128, 5, 128))
    return c


def _rope_tables(origin):
    inv_freq = np.power(np.float32(500000.0), -np.arange(0, 16, 2, dtype=np.float32) / np.float32(16)).astype(np.float32)
    pos = np.maximum(np.arange(SEQ) + origin, 0).astype(np.float32)
    ang = (pos[:, None] * inv_freq[None, :]).astype(np.float32)
    cos = np.cos(ang).astype(np.float32)
    sin = np.sin(ang).astype(np.float32)
    C = np.ones((128, SEQ), np.float32)
    S_ = np.zeros((128, SEQ), np.float32)
    for p in range(128):
        d = p % 64
        if d < 8:
            C[p] = cos[:, d]
            S_[p] = -sin[:, d]
        elif d < 16:
            C[p] = cos[:, d - 8]
            S_[p] = sin[:, d - 8]
    return np.stack([C, S_], 0)


def _prep_shared(inp):
    c = _host_consts()
    w_in = np.asarray(inp["w_in"][0], np.float32)
    offs = np.cumsum((0,) + SPLIT)
    seg = {n: w_in[:, offs[j]:offs[j + 1]] for j, n in enumerate(
        ["qa", "ka", "va", "qb", "kb", "vb", "qi", "ki", "wi", "ga", "gb"])}
    kbp = seg["kb"][:, _rope_perm(128)]
    kip = seg["ki"][:, _rope_perm(64)]
    wk = np.concatenate([seg["ka"], seg["va"], seg["kb"], kbp, seg["vb"], seg["ki"], kip], 1)
    hb_order = np.concatenate([np.r_[j * 64:(j + 1) * 64, (4 + j) * 64:(5 + j) * 64] for j in range(4)])
    qb = seg["qb"][:, hb_order]
    qbp = seg["qb"][:, _rope_perm(512)][:, hb_order]
    qip = seg["qi"][:, _rope_perm(512)]
    wrep = np.repeat(seg["wi"], 16, axis=1)
    pad = np.zeros((D, 384), np.float32)
    wq = np.concatenate([seg["qa"], qb, qbp, seg["qi"], qip, wrep, pad, seg["ga"], seg["gb"]], 1)
    assert wq.shape[1] == 5120 and wk.shape[1] == 1536
    rb = np.asarray(inp["rel_bias"][0], np.float32)
    rbg = rb[:, c["_dist"]]
    rbg = np.ascontiguousarray(np.transpose(rbg, (1, 0, 2, 3))).reshape(128, 8 * 5 * 128)
    gcol = np.stack([np.asarray(inp[k][0], np.float32).reshape(8, 128).T for k in ("n1_g", "n2_g", "n3_g")], 1)
    sh = {
        "w1a": np.ascontiguousarray(inp["ffn1_w_in"][0], np.float32),
        "w1b": np.ascontiguousarray(inp["ffn1_w_out"][0], np.float32),
        "w2a": np.ascontiguousarray(inp["ffn2_w_in"][0], np.float32),
        "w2b": np.ascontiguousarray(inp["ffn2_w_out"][0], np.float32),
        "wk": np.ascontiguousarray(wk), "wq": np.ascontiguousarray(wq),
        "wab": np.ascontiguousarray(np.concatenate([inp["w_branch_a"][0], inp["w_branch_b"][0]], 0), np.float32),
        "wo": np.ascontiguousarray(inp["w_out"][0], np.float32),
        "gcol": np.ascontiguousarray(gcol.reshape(128, 24), np.float32),
        "gf": np.asarray(inp["nf_g"], np.float32).reshape(1, D),
        "rbg": rbg.astype(np.float32), "amask": c["amask"], "emask": c["emask"], "ident": c["ident"], "cpw": c["cpw"],
    }
    return sh


def _prep_core(x, b, hf, sh):
    origin = -256 * (1 - hf)
    xc = np.zeros((SEQ, D), np.float32)
    if hf == 0:
        xc[256:] = x[b, 0:SEQ - 256]
    else:
        xc[:] = x[b]
    kb = np.zeros((128, 32), np.float32)
    kbrow = np.zeros((128, 256), np.float32)
    if hf == 0:
        kb[:, 0:2] = NEG
        kbrow[:] = -200.0
    m = dict(sh)
    m.update({"x": xc, "cs": _rope_tables(origin), "kb": kb, "kbrow": kbrow})
    return m


def kernel(**inputs):
    x = np.asarray(inputs["x"], np.float32)
    sh = _prep_shared(inputs)
    in_maps = [_prep_core(x, c // 2, c % 2, sh) for c in range(8)]
    nc = build(NT)
    res = run_bass_kernel_spmd(nc, in_maps, core_ids=list(range(8)))
    out = np.empty((4, SEQ, D), np.float32)
    for c in range(8):
        b, hf = c // 2, c % 2
        y = np.asarray(res.results[c]["y"], np.float32).reshape(NT, 256, D)
        for i in range(NT):
            p0 = 512 * i + 256 * hf
            out[b, p0:p0 + 256] = y[i]
    return out
```

```python
import contextlib
import numpy as np
import concourse.bass as bass
import concourse.mybir as mybir
from concourse.bass_utils import run_bass_kernel_spmd

F32 = mybir.dt.float32
BF16 = mybir.dt.bfloat16
AF = mybir.ActivationFunctionType
ALU = mybir.AluOpType

D = 1024
DFF = 2816
SEQ = 4096
NT = 8
TILE = 512
NIT = 16
NEG = -30000.0
NS = 5
NBLK = 80


class Res:
    __slots__ = ("name", "w", "r", "sem", "semcnt")

    def __init__(self, name):
        self.name = name
        self.w = None
        self.r = []
        self.sem = None
        self.semcnt = 0


class Prog:
    COMPUTE = ("pe", "act", "dve", "pool")

    def __init__(self, nc, stack):
        self.nc = nc
        self.stack = stack
        self.ops = {e: [] for e in ("pe", "act", "dve", "pool", "sp")}
        self.sems = {}
        self.cnt = {}
        for e in self.COMPUTE:
            self.sems[e] = stack.enter_context(nc.semaphore("s_" + e))
            self.cnt[e] = 0
        self.known = {e: {} for e in self.ops}
        self.ndsem = 0

    def res(self, name):
        return Res(name)

    def _res_sem(self, r):
        if r.sem is None:
            key = "d%d" % self.ndsem
            self.sems[key] = self.stack.enter_context(self.nc.semaphore("sd_%d" % self.ndsem))
            self.ndsem += 1
            r.sem = key
        return r.sem

    def _deps(self, eng, R, W, is_dma):
        waits = {}

        def need(ev, kind):
            if ev is None:
                return
            key, val, e2 = ev
            if (not is_dma) and e2 == eng and key == eng:
                if eng == "pe":
                    return
            if self.known[eng].get(key, 0) >= val:
                return
            if waits.get(key, 0) < val:
                waits[key] = val

        for r in R:
            need(r.w, "raw")
        for r in W:
            need(r.w, "waw")
            for ev in r.r:
                need(ev, "war")
        for k, v in waits.items():
            self.known[eng][k] = v
        return list(waits.items())

    def op(self, eng, fn, R=(), W=(), inc=True):
        waits = self._deps(eng, R, W, False)
        if inc:
            self.cnt[eng] += 1
            ev = (eng, self.cnt[eng], eng)
        else:
            ev = (eng, self.cnt[eng] + 1, eng)
        for r in R:
            r.r.append(ev)
        for r in W:
            r.w = ev
            r.r = []
        self.ops[eng].append((waits, fn, (eng, 1) if inc else None))

    def dma(self, q, out, in_, R=(), W=(), owner=None, **kw):
        if owner is None:
            owner = (list(W) + list(R))[0]
        waits = self._deps(q, R, W, True)
        key = self._res_sem(owner)
        owner.semcnt += 16
        ev = (key, owner.semcnt, "dma")
        for r in R:
            r.r.append(ev)
        for r in W:
            r.w = ev
            r.r = []
        self.ops[q].append((waits, lambda e: e.dma_start(out=out, in_=in_, **kw), (key, 16)))

    def final_wait(self, eng, resources):
        waits = {}
        for r in resources:
            evs = ([r.w] if r.w else []) + list(r.r)
            for key, val, _ in evs:
                if waits.get(key, 0) < val:
                    waits[key] = val
        self.ops[eng].append((list(waits.items()), None, None))

    def emit(self):
        nc = self.nc
        sems = self.sems

        def replay(name):
            def body(e):
                for waits, fn, inc in self.ops[name]:
                    for k, v in waits:
                        e.wait_ge(sems[k], v)
                    if fn is None:
                        continue
                    ins = fn(e)
                    if inc is not None:
                        ins.then_inc(sems[inc[0]], inc[1])
            return body

        with nc.Block() as block:
            block.sync(replay("sp"))
            block.tensor(replay("pe"))
            block.scalar(replay("act"))
            block.vector(replay("dve"))
            block.gpsimd(replay("pool"))


def build(n_tiles=NT, stop_after=None, use_scratch=True):
    nc = bass.Bass("TRN2", target_bir_lowering=False)

    def din(name, shape):
        return nc.dram_tensor(name, list(shape), F32, kind="ExternalInput").ap()

    x_d = din("x", [SEQ, D])
    cs_d = din("cs", [2, 128, SEQ])
    kb_d = din("kb", [128, 32])
    kbrow_d = din("kbrow", [128, 256])
    w1a_d = din("w1a", [D, 2 * DFF])
    w1b_d = din("w1b", [DFF, D])
    w2a_d = din("w2a", [D, 2 * DFF])
    w2b_d = din("w2b", [DFF, D])
    wk_d = din("wk", [D, 1536])
    wq_d = din("wq", [D, 5120])
    wab_d = din("wab", [D, D])
    wo_d = din("wo", [D, D])
    gcol_d = din("gcol", [128, 24])
    gf_d = din("gf", [1, D])
    rbg_d = din("rbg", [128, 8 * 5 * 128])
    amask_d = din("amask", [128, 5 * 128])
    emask_d = din("emask", [128, 8 * 128])
    ident_d = din("ident", [128, 128])
    cpw_d = din("cpw", [128, NIT + 1])
    y_d = nc.dram_tensor("y", [n_tiles * 256, D], F32, kind="ExternalOutput").ap()
    wscr_d = nc.dram_tensor("wscr", [NBLK, 128, 4096], BF16, kind="Internal").ap() if (use_scratch and n_tiles > 1) else None

    with contextlib.ExitStack() as st:
        P = Prog(nc, st)

        def sb(name, shape, dt):
            return st.enter_context(nc.sbuf_tensor(name, list(shape), dt))

        xt = sb("xt", [128, 4, D], F32)
        hTa = sb("hT", [128, 8, TILE], BF16)
        hTb = hTa
        big = sb("big", [128, 12288], BF16)
        aT = big[:, 0:11264].rearrange("p (a b) -> p a b", a=22)
        hb = aT[:, 0:8, :].rearrange("p a b -> p (a b)").rearrange("p (j f) -> p j f", j=4)
        sg = [sb("sg%d" % i, [128, TILE], F32) for i in range(2)]
        kAT = sb("kAT", [128, 4, 1024], BF16)
        vA = sb("vA", [128, 8, 8, 66], BF16)
        kBT = sb("kBT", [128, SEQ], BF16)
        vB = sb("vB", [128, 32, 2, 66], BF16)
        kiT = sb("kiT", [64, SEQ], BF16)
        qAT = [sb("qAT%d" % i, [128, 4, 256], BF16) for i in range(2)]
        qBT = [sb("qBT%d" % i, [128, 4, 256], BF16) for i in range(2)]
        qiT = sb("qiT", [64, 2, 8, 8, 16], BF16)
        wrepT = sb("wrepT", [128, 256], F32)
        score = big[:, 0:8192].bitcast(F32)
        mb = big[:, 8192:12288]
        PT = [sb("PT%d" % i, [128, 8, 128], BF16) for i in range(3)]
        jk = sb("jk", [128, 2 * SEQ], BF16)
        xn = jk[:, :].bitcast(F32).rearrange("p (b f) -> p b f", b=4)
        sqj = PT[0][:, :, :].rearrange("p a b -> p (a b)")
        rt = sg
        cst = sb("cst", [128, 2, TILE], F32)
        biasT = sb("biasT", [128, 8, 5, 128], BF16)
        emask = sb("emask_s", [128, 8, 128], BF16)
        ident = sb("ident_s", [128, 128], BF16)
        gfb = sb("gfb", [128, D], F32)
        gcol = sb("gcol_s", [128, 24], F32)
        kbt = sb("kbt", [128, 32], F32)
        kbrow = sb("kbrow_s", [128, 256], F32)
        cpw = sb("cpw_s", [128, NIT + 1], F32)
        oab = [sb("oab%d" % i, [128, 512], BF16) for i in range(2)]
        oT = sb("oT", [128, 8, 256], BF16)
        mT = sb("mT", [128, 8, 256], BF16)
        wsel = sb("wsel", [128, 8, 128], BF16)
        rl = [sb("rl%d" % i, [128, TILE], BF16) for i in range(2)]
        st_s = sb("stat", [128, 96], F32)
        wsl = [sb("wsl%d" % i, [128, 4096], BF16) for i in range(NS)]
        banks = [st.enter_context(nc.psum_tensor("bk%d" % i, [128, 512], F32)) for i in range(8)]

        R = {}

        def rs(name):
            if name not in R:
                R[name] = P.res(name)
            return R[name]

        Rbank = [P.res("bank%d" % i) for i in range(8)]
        Rwsl = [P.res("wsl%d" % i) for i in range(NS)]
        state = {"bank": 0, "wsl": 0, "alt": 0, "pt": 0, "rl": 0, "sg": 0, "rt": 0, "wblk": 0, "tile": 0}
        Rwst = [P.res("wst%d" % i) for i in range(NS)]
        Rwslh = [P.res("wslh%d" % i) for i in range(NS)]
        Rwready = [P.res("wready%d" % i) for i in range(NS)]

        def bank():
            i = state["bank"]
            state["bank"] = (i + 1) % 6
            return banks[i], Rbank[i]

        def wslot():
            i = state["wsl"]
            state["wsl"] = (i + 1) % NS
            return wsl[i], Rwsl[i]

        def rot(key, n):
            i = state[key]
            state[key] = (i + 1) % n
            return i

        pend_st = []

        def flush_stores():
            for (k_, flat, r, n_el) in pend_st:
                P.dma("sp", wscr_d[k_, :, 0:n_el], flat, R=[r], owner=Rwst[Rwsl.index(r)])
            del pend_st[:]

        def load_w(src2d, kc0, nkc, c0, ncols, slot=None, soff=0):
            if slot is None:
                slot = wslot()
            t, r = slot
            if pend_st and pend_st[0][2] is not r:
                flush_stores()
            n_el = nkc * ncols
            flat = t[:, soff:soff + n_el]
            dst = flat.rearrange("p (k c) -> p k c", k=nkc)
            k_ = state["wblk"]
            state["wblk"] += 1
            assert k_ < NBLK
            if wscr_d is not None and state["tile"] >= 1:
                P.dma("sp", flat, wscr_d[k_, :, 0:n_el], R=Rwready, W=[r], owner=Rwslh[Rwsl.index(r)])
            else:
                src = src2d[kc0 * 128:(kc0 + nkc) * 128, c0:c0 + ncols].rearrange("(k p) c -> p k c", p=128)
                P.dma("pool", dst, src, W=[r])
                if wscr_d is not None:
                    pend_st.append((k_, flat, r, n_el))
            return dst, r

        def load_pair(src2d, c0a, c0b, ncols):
            slot = wslot()
            t, r = slot
            if pend_st and pend_st[0][2] is not r:
                flush_stores()
            n_el = 8 * ncols
            k_ = state["wblk"]
            state["wblk"] += 1
            assert k_ < NBLK
            va = t[:, 0:n_el].rearrange("p (k c) -> p k c", k=8)
            vb = t[:, n_el:2 * n_el].rearrange("p (k c) -> p k c", k=8)
            if wscr_d is not None and state["tile"] >= 1:
                P.dma("sp", t[:, 0:2 * n_el], wscr_d[k_, :, 0:2 * n_el], R=Rwready, W=[r], owner=Rwslh[Rwsl.index(r)])
            else:
                for dst, c0 in ((va, c0a), (vb, c0b)):
                    src = src2d[0:1024, c0:c0 + ncols].rearrange("(k p) c -> p k c", p=128)
                    P.dma("pool", dst, src, W=[r])
                if wscr_d is not None:
                    pend_st.append((k_, t[:, 0:2 * n_el], r, 2 * n_el))
            return va, vb, r

        def mm(out, lhsT, rhs, start, stop, Rr, Ww, last, skip=False):
            P.op("pe", lambda e: e.matmul(out, lhsT, rhs, start=start, stop=stop, skip_group_check=skip), R=Rr, W=Ww, inc=last)

        def evac(out, in_, Rr, Ww, eng=None):
            if eng is None:
                eng = "act" if (state["alt"] % 2 == 0) else "dve"
                state["alt"] += 1
            if eng == "act":
                P.op("act", lambda e: e.activation(out=out, in_=in_, func=AF.Copy), R=Rr, W=Ww)
            else:
                P.op("dve", lambda e: e.tensor_copy(out, in_), R=Rr, W=Ww)

        P.dma("sp", xn, x_d[0:512, :].rearrange("(b p) f -> p b f", p=128),
              W=[rs("xn%d" % b) for b in range(4)] + [rs("jk"), rs("jka")], owner=rs("xnload"))
        P.dma("sp", cst[:, :, :], cs_d[:, :, 0:512].rearrange("t p n -> p t n"), W=[rs("cst")])
        Rc = rs("consts")
        P.dma("sp", gcol[:], gcol_d, W=[Rc])
        P.dma("sp", kbt[:], kb_d, W=[Rc])
        P.dma("sp", kbrow[:], kbrow_d, W=[Rc])
        P.dma("sp", cpw[:], cpw_d, W=[Rc])
        P.dma("sp", gfb[:], bass.AP(gf_d.tensor, 0, [[0, 128], [1, D]]), W=[Rc])
        Rc2 = rs("consts2")
        P.dma("pool", ident[:], ident_d, W=[Rc2])
        P.dma("pool", emask[:].rearrange("p a b -> p (a b)"), emask_d, W=[Rc2])
        Rsc = rs("score")
        Rbias = rs("biasT")
        P.dma("sp", score[:, 2560:3200], amask_d, W=[Rsc])
        for half in range(2):
            P.dma("sp", score[:, 0:2560], rbg_d[:, half * 2560:(half + 1) * 2560], W=[Rsc])
            tv = score[:, 0:2560].rearrange("p (h r) -> p h r", h=4)
            am = score[:, 2560:3200].unsqueeze(1).to_broadcast([128, 4, 640])
            P.op("dve", lambda e, tv=tv, am=am: e.tensor_tensor(out=tv, in0=tv, in1=am, op=ALU.add), R=[Rsc], W=[Rsc])
            bo = biasT[:, half * 4:(half + 1) * 4, :, :].rearrange("p h r q -> p h (r q)")
            P.op("dve", lambda e, tv=tv, bo=bo: e.tensor_scalar(out=bo, in0=tv, scalar1=8.0, scalar2=None, op0=ALU.mult),
                 R=[Rsc], W=[Rbias])
        Rva = [rs("vA%d" % j) for j in range(8)]
        RvB = [rs("vB%d" % j) for j in range(32)]
        for t_ in (qAT[0], qAT[1], qBT[0], qBT[1]):
            P.op("pool", lambda e, t_=t_: e.memset(t_[:, :, :], 0.0), W=[rs("qAT"), rs("qBT")])
        P.op("pool", lambda e: e.memset(vA[:, :, :, 64:66], 1.0), W=Rva)
        P.op("pool", lambda e: e.memset(vB[:, :, :, 64:66], 1.0), W=RvB)

        Rxt = [rs("xt%d" % b) for b in range(4)]
        Rxn = [rs("xn%d" % b) for b in range(4)]
        Rhb = [rs("aT")] * 4
        Rstat = rs("stat")
        RkAT = [rs("kAT%d" % j) for j in range(2)]
        RkB = [rs("kB%d" % j) for j in range(8)]
        Rki = [rs("ki%d" % j) for j in range(8)]

        def rmsnorm_to_hT(blocks, gi, hT, RhT, col0, from_xn=False):
            nb = len(blocks)
            xsrc = xn if from_xn else xt
            Rsrc = (lambda b: [Rxn[b], rs("jk"), rs("jka")]) if from_xn else (lambda b: [Rxt[b]])
            P.op("dve", lambda e: e.memset(st_s[:, 0:8], 0.0), W=[Rstat])
            for j, b in enumerate(blocks):
                P.op("act", lambda e, b=b, j=j: e.activation(out=sqj[:], in_=xsrc[:, b, :], func=AF.Square,
                                                             accum_out=st_s[:, j:j + 1]),
                     R=Rsrc(b) + [Rstat], W=[rs("PT0"), Rstat])
            P.op("dve", lambda e: e.tensor_scalar(out=st_s[:, 4:4 + nb], in0=st_s[:, 0:nb], scalar1=1.0 / D, scalar2=1e-6,
                                                  op0=ALU.mult, op1=ALU.add), R=[Rstat], W=[Rstat])
            P.op("act", lambda e: e.activation(out=st_s[:, 4:4 + nb], in_=st_s[:, 4:4 + nb], func=AF.Sqrt), R=[Rstat], W=[Rstat])
            P.op("dve", lambda e: e.reciprocal(st_s[:, 4:4 + nb], st_s[:, 4:4 + nb]), R=[Rstat], W=[Rstat])
            for j, b in enumerate(blocks):
                P.op("dve", lambda e, b=b, j=j: e.tensor_scalar(out=hb[:, j, :], in0=xsrc[:, b, :], scalar1=st_s[:, 4 + j:5 + j],
                                                               scalar2=None, op0=ALU.mult),
                     R=Rsrc(b) + [Rstat], W=[Rhb[j]])
            for kc in range(8):
                bk, rb = bank()
                for j in range(nb):
                    mm(bk[:, j * 128:(j + 1) * 128], hb[:, j, kc * 128:(kc + 1) * 128], ident[:], True, True,
                       [Rhb[j], Rc2], [rb], j == nb - 1)
                o = hT[:, kc, col0:col0 + nb * 128]
                i_ = bk[:, 0:nb * 128]
                gs = gcol[:, gi * 8 + kc:gi * 8 + kc + 1]
                if kc % 2 == 0:
                    P.op("dve", lambda e, o=o, i_=i_, gs=gs: e.tensor_scalar(out=o, in0=i_, scalar1=gs, scalar2=None, op0=ALU.mult),
                         R=[rb, Rc], W=[RhT])
                else:
                    P.op("act", lambda e, o=o, i_=i_, gs=gs: e.activation(out=o, in_=i_, func=AF.Identity, scale=gs),
                         R=[rb, Rc], W=[RhT])

        def ffn(hT, RhT, col0, ntok, blocks, wa_d, wb_d, from_xn=False):
            RaT = rs("aT")
            for g in range(11):
                wg, wu, rw = load_pair(wa_d, g * 256, DFF + g * 256, 256)
                for jj in range(2):
                    j = g * 2 + jj
                    bg, rbg_ = bank()
                    bu, rbu = bank()
                    for kc in range(8):
                        mm(bg[:, 0:ntok], wg[:, kc, jj * 128:(jj + 1) * 128], hT[:, kc, col0:col0 + ntok], kc == 0, kc == 7,
                           [rw, RhT], [rbg_], kc == 7)
                    for kc in range(8):
                        mm(bu[:, 0:ntok], wu[:, kc, jj * 128:(jj + 1) * 128], hT[:, kc, col0:col0 + ntok], kc == 0, kc == 7,
                           [rw, RhT], [rbu], kc == 7)
                    si = rot("sg", 2)
                    rsg = rs("sg%d" % si)
                    P.op("act", lambda e, si=si, bg=bg: e.activation(out=sg[si][:, 0:ntok], in_=bg[:, 0:ntok], func=AF.Silu),
                         R=[rbg_], W=[rsg])
                    P.op("dve", lambda e, si=si, bu=bu, j=j: e.tensor_tensor(out=aT[:, j, 0:ntok], in0=sg[si][:, 0:ntok],
                                                                             in1=bu[:, 0:ntok], op=ALU.mult),
                         R=[rsg, rbu], W=[RaT])
            for ch in range(2):
                pieces = []
                for (k0, nk) in ((0, 8), (8, 8), (16, 6)):
                    pieces.append(load_w(wb_d, k0, nk, ch * 512, 512))
                for j, b in enumerate(blocks):
                    bk, rb = bank()
                    for kc in range(22):
                        wp, rw = pieces[kc // 8]
                        mm(bk[:, :], aT[:, kc, j * 128:(j + 1) * 128], wp[:, kc % 8, :], kc == 0, kc == 21,
                           [RaT, rw], [rb], kc == 21)
                    xs = xt[:, b, ch * 512:(ch + 1) * 512]
                    xi = xn[:, b, ch * 512:(ch + 1) * 512] if from_xn else xs
                    Ri = [Rxn[b], rs("jk"), rs("jka")] if from_xn else [Rxt[b]]
                    P.op("dve", lambda e, xs=xs, xi=xi, bk=bk: e.scalar_tensor_tensor(out=xs, in0=bk[:, :], scalar=0.5, in1=xi,
                                                                                      op0=ALU.mult, op1=ALU.add),
                         R=[rb] + Ri, W=[Rxt[b]])

        def rope(out, bt, btp, np_, cols, c0, Rr, Ww, psplit=False):
            i1, i2 = 0, 1
            r1, r2 = rs("sg0"), rs("sg1")
            n = 1
            for s_ in bt.shape[1:]:
                n *= s_
            rep = n // cols
            if rep == 1:
                C = cst[0:np_, 0, c0:c0 + cols]
                S_ = cst[0:np_, 1, c0:c0 + cols]
                t1 = rt[i1][0:np_, 0:n]
                t2 = rt[i2][0:np_, 0:n]
            else:
                C = cst[0:np_, 0, c0:c0 + cols].unsqueeze(1).to_broadcast([np_, rep, cols])
                S_ = cst[0:np_, 1, c0:c0 + cols].unsqueeze(1).to_broadcast([np_, rep, cols])
                t1 = rt[i1][0:np_, 0:n].rearrange("p (a b) -> p a b", a=rep)
                t2 = rt[i2][0:np_, 0:n].rearrange("p (a b) -> p a b", a=rep)
            P.op("dve", lambda e: e.tensor_tensor(out=t1, in0=bt, in1=C, op=ALU.mult), R=Rr + [rs("cst")], W=[r1])
            P.op("dve", lambda e: e.tensor_tensor(out=t2, in0=btp, in1=S_, op=ALU.mult), R=Rr + [rs("cst")], W=[r2])
            if psplit:
                for (o_, p0_, p1_) in out:
                    P.op("pool", lambda e, o_=o_, p0_=p0_, p1_=p1_: e.tensor_tensor(out=o_, in0=t1[p0_:p1_], in1=t2[p0_:p1_], op=ALU.add),
                         R=[r1, r2], W=Ww)
            elif isinstance(out, list):
                for (o_, c_, n_, a_) in out:
                    a1 = rt[i1][0:np_, c_:c_ + n_].rearrange("p (a b) -> p a b", a=a_)
                    a2 = rt[i2][0:np_, c_:c_ + n_].rearrange("p (a b) -> p a b", a=a_)
                    P.op("pool", lambda e, o_=o_, a1=a1, a2=a2: e.tensor_tensor(out=o_, in0=a1, in1=a2, op=ALU.add), R=[r1, r2], W=Ww)
            else:
                P.op("pool", lambda e: e.tensor_tensor(out=out, in0=t1, in1=t2, op=ALU.add), R=[r1, r2], W=Ww)

        def kside(i, part):
            RhT = rs("hT")
            slot_j = i % 2
            if part == "early":
                yield from kside_early(i)
                return
            w0, rw0 = load_w(wk_d, 0, 8, 0, 512)
            for c in range(4):
                bk, rb = bank()
                for kc in range(8):
                    mm(bk[:, :], w0[:, kc, c * 128:(c + 1) * 128], hTb[:, kc, :], kc == 0, kc == 7, [rw0, RhT], [rb], kc == 7)
                evac(kAT[:, c, slot_j * 512:(slot_j + 1) * 512], bk[:, :], [rb], [RkAT[slot_j]])
            yield
            w1, rw1 = load_w(wk_d, 0, 8, 512, 512)
            for b in range(4):
                bk, rb = bank()
                for kc in range(8):
                    mm(bk[:, :], hTb[:, kc, b * 128:(b + 1) * 128], w1[:, kc, :], kc == 0, kc == 7, [RhT, rw1], [rb], kc == 7)
                rt_ = (4 * i + b) % 8
                evac(vA[:, rt_, :, 0:64], bk[:, :].rearrange("p (h d) -> p h d", h=8), [rb], [Rva[rt_]])
                if b % 2 == 1:
                    yield

        def kside_early(i):
            RhT = rs("hT")
            w2, rw2 = load_w(wk_d, 0, 8, 1024, 512)
            b1, rb1 = bank()
            b2, rb2 = bank()
            for kc in range(8):
                mm(b1[:, :], w2[:, kc, 0:128], hTb[:, kc, :], kc == 0, kc == 7, [rw2, RhT], [rb1], kc == 7)
            for kc in range(8):
                mm(b2[:, :], w2[:, kc, 128:256], hTb[:, kc, :], kc == 0, kc == 7, [rw2, RhT], [rb2], kc == 7)
            rope(kBT[:, i * 512:(i + 1) * 512], b1[:, :], b2[:, :], 128, 512, 0, [rb1, rb2], [RkB[i]])
            b3, rb3 = bank()
            for b in range(4):
                for kc in range(8):
                    mm(b3[:, b * 128:(b + 1) * 128], hTb[:, kc, b * 128:(b + 1) * 128], w2[:, kc, 256:384], kc == 0, kc == 7,
                       [RhT, rw2], [rb3], (kc == 7 and b == 3))
            evac(vB[:, 4 * i:4 * i + 4, :, 0:64], b3[:, :].rearrange("p (b g d) -> p b g d", b=4, g=2), [rb3],
                 [RvB[4 * i + b] for b in range(4)])
            b4, rb4 = bank()
            b5, rb5 = bank()
            for kc in range(8):
                mm(b4[0:64, :], w2[:, kc, 384:448], hTb[:, kc, :], kc == 0, kc == 7, [rw2, RhT], [rb4], kc == 7)
            for kc in range(8):
                mm(b5[0:64, :], w2[:, kc, 448:512], hTb[:, kc, :], kc == 0, kc == 7, [rw2, RhT], [rb5], kc == 7)
            rope(kiT[0:64, i * 512:(i + 1) * 512], b4[0:64, :], b5[0:64, :], 64, 512, 0, [rb4, rb5], [Rki[i]])
            yield

        def qside(part):
            RhT = rs("hT")
            h2o = lambda kc: hTb[:, kc, 256:512]
            if part == "early":
                yield from qside_early()
                return
            w0, rw0 = load_w(wq_d, 0, 8, 0, 512)
            for c in range(4):
                bk, rb = bank()
                for kc in range(8):
                    mm(bk[:, 0:256], w0[:, kc, c * 128:(c + 1) * 128], h2o(kc), kc == 0, kc == 7, [rw0, RhT], [rb], kc == 7)
                evac(qAT[0][0:64, c, :], bk[0:64, 0:256], [rb], [rs("qAT")])
                evac(qAT[1][64:128, c, :], bk[64:128, 0:256], [rb], [rs("qAT")])
            yield
            w1, rw1 = load_w(wq_d, 0, 8, 512, 512)
            w2, rw2 = load_w(wq_d, 0, 8, 1024, 512)
            for cp in range(2):
                bt, rbt = bank()
                bp, rbp = bank()
                for cc in range(2):
                    c = cp * 2 + cc
                    for kc in range(8):
                        mm(bt[:, cc * 256:(cc + 1) * 256], w1[:, kc, c * 128:(c + 1) * 128], h2o(kc), kc == 0, kc == 7,
                           [rw1, RhT], [rbt], (kc == 7 and cc == 1))
                for cc in range(2):
                    c = cp * 2 + cc
                    for kc in range(8):
                        mm(bp[:, cc * 256:(cc + 1) * 256], w2[:, kc, c * 128:(c + 1) * 128], h2o(kc), kc == 0, kc == 7,
                           [rw2, RhT], [rbp], (kc == 7 and cc == 1))
                outs = [(qBT[0][0:64, cp * 2:cp * 2 + 2, :], 0, 64), (qBT[1][64:128, cp * 2:cp * 2 + 2, :], 64, 128)]
                rope(outs, bt[:, :].rearrange("p (a b) -> p a b", a=2),
                     bp[:, :].rearrange("p (a b) -> p a b", a=2), 128, 256, 256, [rbt, rbp], [rs("qBT")], psplit=True)
                yield

        def qside_early():
            RhT = rs("hT")
            h2o = lambda kc: hTb[:, kc, 256:512]
            w3, rw3 = load_w(wq_d, 0, 8, 1536, 512)
            w4, rw4 = load_w(wq_d, 0, 8, 2048, 512)
            for hp in range(4):
                bt, rbt = bank()
                bp, rbp = bank()
                for hh in range(2):
                    h = hp * 2 + hh
                    for kc in range(8):
                        mm(bt[0:64, hh * 256:(hh + 1) * 256], w3[:, kc, h * 64:(h + 1) * 64], h2o(kc), kc == 0, kc == 7,
                           [rw3, RhT], [rbt], (kc == 7 and hh == 1))
                for hh in range(2):
                    h = hp * 2 + hh
                    for kc in range(8):
                        mm(bp[0:64, hh * 256:(hh + 1) * 256], w4[:, kc, h * 64:(h + 1) * 64], h2o(kc), kc == 0, kc == 7,
                           [rw4, RhT], [rbp], (kc == 7 and hh == 1))
                outs = [(qiT[0:64, bl, :, hp * 2 + hh, :], hh * 256 + bl * 128, 128, 8) for hh in range(2) for bl in range(2)]
                rope(outs, bt[0:64, :].rearrange("p (a b) -> p a b", a=2),
                     bp[0:64, :].rearrange("p (a b) -> p a b", a=2), 64, 256, 256, [rbt, rbp], [rs("qiT")])
            w5, rw5 = load_w(wq_d, 0, 8, 2560, 128)
            bk, rb = bank()
            for kc in range(8):
                mm(bk[:, 0:256], w5[:, kc, :], h2o(kc), kc == 0, kc == 7, [rw5, RhT], [rb], kc == 7)
            evac(wrepT[:, :], bk[:, 0:256], [rb], [rs("wrepT")])
            yield

        def pv_finish(acc_banks, racc, osb, ro, kc0, q0):
            for g in range(2):
                accv = banks[6 + g][:, 0:264].rearrange("p (h d) -> p h d", h=4)
                rec = st_s[:, 16 + 4 * g:20 + 4 * g]
                P.op("dve", lambda e, accv=accv, rec=rec: e.reciprocal(rec, accv[:, :, 64]), R=[racc[g]], W=[Rstat])
                ov = osb[:, g * 256:(g + 1) * 256].rearrange("p (h d) -> p h d", h=4)
                rb_ = rec.unsqueeze(2).to_broadcast([128, 4, 64])
                P.op("dve", lambda e, ov=ov, accv=accv, rb_=rb_: e.tensor_tensor(out=ov, in0=accv[:, :, 0:64], in1=rb_, op=ALU.mult),
                     R=[racc[g], Rstat], W=[ro])
            bk, rb = bank()
            for c in range(4):
                mm(bk[:, c * 128:(c + 1) * 128], osb[:, c * 128:(c + 1) * 128], ident[:], True, True, [ro, Rc2], [rb], c == 3)
            evac(oT[:, kc0:kc0 + 4, q0:q0 + 128], bk[:, :].rearrange("p (c q) -> p c q", c=4), [rb], [rs("oT")])

        Racc = [Rbank[6], Rbank[7]]

        def mixer_a(i, ob):
            q0 = (ob - 2) * 128
            Tq = 4 * i + ob
            tiles = [T for T in range(Tq - 4, Tq + 1) if T >= 0]
            pend = None
            for idx, T in enumerate(tiles):
                rel = T - (Tq - 4)
                col = (T % 8) * 128
                jslot = (T // 4) % 2
                pi = rot("pt", 3)
                rpt = rs("PT%d" % pi)
                for g in range(2):
                    bk, rb = bank()
                    mm(bk[:, :].rearrange("p (h q) -> p h q", h=4), ident[:], biasT[:, g * 4:(g + 1) * 4, rel, :],
                       True, False, [Rc2, Rbias], [rb], False)
                    for hh in range(4):
                        h = g * 4 + hh
                        c, po = h // 2, (h % 2) * 64
                        mm(bk[:, hh * 128:(hh + 1) * 128], kAT[:, c, col:col + 128], qAT[h % 2][:, c, q0:q0 + 128],
                           False, hh == 3, [RkAT[jslot], rs("qAT")], [rb], hh == 3)
                    P.op("act", lambda e, bk=bk, pi=pi, g=g, T=T: e.activation(
                        out=PT[pi][:, g * 4:(g + 1) * 4, :], in_=bk[:, :].rearrange("p (h q) -> p h q", h=4),
                        func=AF.Exp, scale=0.125, bias=kbt[:, T:T + 1]), R=[rb, Rc], W=[rpt])
                if pend is not None:
                    pend()

                def do_pv(T=T, idx=idx, pi=pi, rpt=rpt):
                    for h in range(8):
                        mm(banks[6 + h // 4][:, (h % 4) * 66:(h % 4) * 66 + 66], PT[pi][:, h, :], vA[:, T % 8, h, :],
                           idx == 0 and h % 4 == 0, idx == len(tiles) - 1, [rpt, Rva[T % 8]], [Racc[h // 4]], h % 4 == 3, skip=True)
                pend = do_pv
                yield
            pend()
            pv_finish(None, Racc, oab[0], rs("oab0"), 0, q0)
            yield

        def idx_scores(i, ob):
            q0 = (ob - 2) * 128
            S = 512 * i + 128 * (ob + 1)
            Rw = rs("wsel")
            wb = wrepT[:, q0:q0 + 128].unsqueeze(1).to_broadcast([128, 8, 128])
            P.op("dve", lambda e: e.tensor_tensor(out=wsel[:, :, :], in0=emask[:, :, :], in1=wb, op=ALU.mult),
                 R=[Rc2, rs("wrepT")], W=[Rw])
            cscale = 1.0 / (8.0 * (8.0 ** 0.5))
            for ks in range(0, S, 512):
                n = min(512, S - ks)
                kres = [Rki[ks // 512]]
                ai = 6 + (ks // 512) % 2
                acc, racc = banks[ai], Rbank[ai]
                pend = None
                for g in range(8):
                    bs, rbs = bank()
                    mm(bs[:, 0:n], qiT[0:64, ob - 2, g, :, :].rearrange("p h q -> p (h q)"), kiT[0:64, ks:ks + n], True, True,
                       [rs("qiT")] + kres, [rbs], True)
                    ri = rot("rl", 2)
                    rrl = rs("rl%d" % ri)
                    P.op("act", lambda e, ri=ri, bs=bs, n=n: e.activation(out=rl[ri][:, 0:n], in_=bs[:, 0:n], func=AF.Relu, scale=cscale),
                         R=[rbs], W=[rrl])
                    if pend is not None:
                        pend()

                    def do_acc(g=g, ri=ri, rrl=rrl, n=n):
                        mm(acc[:, 0:n], wsel[:, g, :], rl[ri][:, 0:n], g == 0, g == 7, [Rw, rrl], [racc], g == 7)
                    pend = do_acc
                pend()
                evac(score[:, ks:ks + n], acc[:, 0:n], [racc], [Rsc])
            return S

        def bisect_gen(S):
            Rb = rs("bstat")
            Rcn = rs("bcnt")
            Rsg = rs("bsgn")
            Rjk = rs("jk")
            Rjka = rs("jka")
            split = S >= 1024
            S1 = ((S // 128) // 2) * 128 if split else S
            n2 = S - S1
            P.op("dve", lambda e: e.tensor_tensor(out=score[:, 0:256], in0=score[:, 0:256], in1=kbrow[:, :], op=ALU.add),
                 R=[Rsc, Rc], W=[Rsc])
            P.op("dve", lambda e: e.memset(score[0:64, S - 64:S], -200.0), W=[Rsc])
            sv = score[:, 0:S]
            jv = jk[:, 0:S]
            m8 = st_s[:, 24:32]
            lo0 = st_s[:, 32:33]
            w0 = st_s[:, 33:34]
            cnt = st_s[:, 34:35]
            dd = st_s[:, 35:36]
            mids = [st_s[:, 36:37], st_s[:, 37:38]]
            sgn = st_s[:, 39:40]
            w0c = st_s[:, 40:40 + NIT + 1]
            P.op("dve", lambda e: e.max(out=m8, in_=sv), R=[Rsc], W=[Rb])
            P.op("dve", lambda e: e.tensor_scalar(out=jv, in0=sv, scalar1=-100.0, scalar2=None, op0=ALU.max, op1=ALU.min,
                                                  accum_out=lo0), R=[Rsc, Rb], W=[Rjk, Rjka, Rb])
            P.op("dve", lambda e: e.tensor_tensor(out=w0, in0=m8[:, 0:1], in1=lo0, op=ALU.subtract), R=[Rb], W=[Rb])
            P.op("dve", lambda e: e.tensor_scalar(out=w0, in0=w0, scalar1=1.001, scalar2=1e-4, op0=ALU.mult, op1=ALU.add),
                 R=[Rb], W=[Rb])
            P.op("dve", lambda e: e.tensor_scalar(out=w0c, in0=cpw[:, :], scalar1=w0, scalar2=None, op0=ALU.mult),
                 R=[Rb, Rc], W=[Rb])
            P.op("dve", lambda e: e.tensor_tensor(out=mids[0], in0=lo0, in1=w0c[:, 0:1], op=ALU.add), R=[Rb], W=[Rb])
            yield
            for n in range(NIT):
                mcur, mnext = mids[n % 2], mids[(n + 1) % 2]
                P.op("dve", lambda e, mcur=mcur: e.tensor_scalar(out=jk[:, 0:S1], in0=score[:, 0:S1], scalar1=mcur, scalar2=None,
                                                                 op0=ALU.is_ge, op1=ALU.add, accum_out=cnt),
                     R=[Rsc, Rb], W=[Rjk, Rcn])
                if split:
                    P.op("act", lambda e, mcur=mcur: e.activation(out=jk[:, S1:S], in_=score[:, S1:S], func=AF.Sign, scale=-1.0,
                                                                 bias=mcur, accum_out=sgn), R=[Rsc, Rb], W=[Rjka, Rsg])
                    P.op("dve", lambda e: e.scalar_tensor_tensor(out=cnt, in0=sgn, scalar=-0.5, in1=cnt, op0=ALU.mult, op1=ALU.add),
                         R=[Rsg, Rcn], W=[Rcn])
                tval = 255.5 - n2 / 2.0
                P.op("dve", lambda e, tval=tval: e.tensor_scalar(out=dd, in0=cnt, scalar1=tval, scalar2=0.5, op0=ALU.is_ge,
                                                                 op1=ALU.subtract), R=[Rcn], W=[rs("bdd")])
                P.op("dve", lambda e, mcur=mcur, mnext=mnext, n=n: e.scalar_tensor_tensor(
                    out=mnext, in0=dd, scalar=w0c[:, n:n + 1], in1=mcur, op0=ALU.mult, op1=ALU.add), R=[rs("bdd"), Rb], W=[Rb])
                yield

        def bisect_final(S):
            Rb = rs("bstat")
            thr = st_s[:, 38:39]
            mfin = st_s[:, 36 + NIT % 2:37 + NIT % 2]
            w0c = st_s[:, 40:40 + NIT + 1]
            P.op("dve", lambda e: e.tensor_tensor(out=thr, in0=mfin, in1=w0c[:, NIT:NIT + 1], op=ALU.subtract), R=[Rb], W=[Rb])
            P.op("dve", lambda e: e.tensor_scalar(out=mb[:, 0:S], in0=score[:, 0:S], scalar1=thr, scalar2=8.0 * NEG, op0=ALU.is_lt,
                                                  op1=ALU.mult), R=[Rsc, Rb], W=[rs("mb")])

        def run_interleaved(gens):
            gens = list(gens)
            while gens:
                for g in list(gens):
                    try:
                        next(g)
                    except StopIteration:
                        gens.remove(g)

        def chain(*gs):
            for g in gs:
                yield from g

        def mixer_b(i, ob):
            q0 = (ob - 2) * 128
            S = 512 * i + 128 * (ob + 1)
            nT = S // 128
            Rmb = rs("mb")
            idb = ident[:, :].unsqueeze(1).to_broadcast([128, 4, 128])
            pend = None
            for T in range(nT):
                pi = rot("pt", 3)
                rpt = rs("PT%d" % pi)
                for g in range(2):
                    bk, rb = bank()
                    bv = bk[:, :].rearrange("p (h q) -> p h q", h=4)
                    mm(bv, kBT[:, T * 128:(T + 1) * 128], qBT[g][:, :, q0:q0 + 128], True, False,
                       [RkB[T // 4], rs("qBT")], [rb], False)
                    mm(bv, mb[:, T * 128:(T + 1) * 128], idb, False, True, [Rmb, Rc2], [rb], True)
                    P.op("act", lambda e, bv=bv, pi=pi, g=g: e.activation(out=PT[pi][:, g * 4:(g + 1) * 4, :], in_=bv,
                                                                          func=AF.Exp, scale=0.125), R=[rb], W=[rpt])
                if pend is not None:
                    pend()

                def do_pv(T=T, pi=pi, rpt=rpt):
                    for h in range(8):
                        mm(banks[6 + h // 4][:, (h % 4) * 66:(h % 4) * 66 + 66], PT[pi][:, h, :], vB[:, T, h // 4, :],
                           T == 0 and h % 4 == 0, T == nT - 1, [rpt, RvB[T]], [Racc[h // 4]], h % 4 == 3, skip=True)
                pend = do_pv
                yield
            pend()
            pv_finish(None, Racc, oab[1], rs("oab1"), 4, q0)
            yield

        sgab = xt[:, 0:2, :].rearrange("p a b -> p (a b)").bitcast(BF16).rearrange("p (c t) -> p c t", c=16)

        def gates_gen():
            RhT = rs("hT")
            for half in range(2):
                wga, rwga = load_w(wq_d, 0, 8, 3072 + half * 512, 512)
                wgb, rwgb = load_w(wq_d, 0, 8, 4096 + half * 512, 512)
                for cc in range(4):
                    c = half * 4 + cc
                    bg, rbg_ = bank()
                    for kc in range(8):
                        mm(bg[:, 0:256], wga[:, kc, cc * 128:(cc + 1) * 128], hTb[:, kc, 256:512], kc == 0, kc == 7,
                           [rwga, RhT], [rbg_], False)
                    for kc in range(8):
                        mm(bg[:, 256:512], wgb[:, kc, cc * 128:(cc + 1) * 128], hTb[:, kc, 256:512], kc == 0, kc == 7,
                           [rwgb, RhT], [rbg_], kc == 7)
                    P.op("act", lambda e, c=c, bg=bg: e.activation(out=sgab[:, 2 * c:2 * c + 2, :],
                                                                   in_=bg[:, :].rearrange("p (a t) -> p a t", a=2), func=AF.Sigmoid),
                         R=[rbg_], W=[Rxt[0], Rxt[1]])
                    yield

        def merge_and_out():
            RhT = rs("hT")
            RmT = rs("mT")
            for half in range(2):
                wab, rwab = load_w(wab_d, 0, 8, half * 512, 512)
                for cc in range(4):
                    c = half * 4 + cc
                    bp, rbp = bank()
                    for kc in range(4):
                        mm(bp[:, 0:256], wab[:, kc, cc * 128:(cc + 1) * 128], oT[:, kc, :], kc == 0, kc == 3,
                           [rwab, rs("oT")], [rbp], False)
                    for kc in range(4, 8):
                        mm(bp[:, 256:512], wab[:, kc, cc * 128:(cc + 1) * 128], oT[:, kc, :], kc == 4, kc == 7,
                           [rwab, rs("oT")], [rbp], kc == 7)
                    si = rot("sg", 2)
                    rsg = rs("sg%d" % si)
                    gv = sgab[:, 2 * c:2 * c + 2, :].rearrange("p a t -> p (a t)")
                    P.op("dve", lambda e, si=si, bp=bp, gv=gv: e.tensor_tensor(out=sg[si][:, :], in0=gv, in1=bp[:, :], op=ALU.mult),
                         R=[Rxt[0], Rxt[1], rbp], W=[rsg])
                    P.op("dve", lambda e, si=si, c=c: e.tensor_tensor(out=mT[:, c, :], in0=sg[si][:, 0:256], in1=sg[si][:, 256:512],
                                                                       op=ALU.add), R=[rsg], W=[RmT])
            for ch in range(2):
                wo, rwo = load_w(wo_d, 0, 8, ch * 512, 512)
                for j in range(2):
                    b = 2 + j
                    bk, rb = bank()
                    for kc in range(8):
                        mm(bk[:, :], mT[:, kc, j * 128:(j + 1) * 128], wo[:, kc, :], kc == 0, kc == 7, [RmT, rwo], [rb], kc == 7)
                    xs = xt[:, b, ch * 512:(ch + 1) * 512]
                    P.op("dve", lambda e, xs=xs, bk=bk: e.tensor_tensor(out=xs, in0=bk[:, :], in1=xs, op=ALU.add),
                         R=[rb, Rxt[b]], W=[Rxt[b]])

        def final_out(i):
            P.op("dve", lambda e: e.memset(st_s[:, 0:8], 0.0), W=[Rstat])
            for j in range(2):
                b = 2 + j
                P.op("act", lambda e, b=b, j=j: e.activation(out=sqj[:], in_=xt[:, b, :], func=AF.Square, accum_out=st_s[:, j:j + 1]),
                     R=[Rxt[b], Rstat], W=[rs("PT0"), Rstat])
            P.op("dve", lambda e: e.tensor_scalar(out=st_s[:, 4:6], in0=st_s[:, 0:2], scalar1=1.0 / D, scalar2=1e-6,
                                                  op0=ALU.mult, op1=ALU.add), R=[Rstat], W=[Rstat])
            P.op("act", lambda e: e.activation(out=st_s[:, 4:6], in_=st_s[:, 4:6], func=AF.Sqrt), R=[Rstat], W=[Rstat])
            P.op("dve", lambda e: e.reciprocal(st_s[:, 4:6], st_s[:, 4:6]), R=[Rstat], W=[Rstat])
            for j in range(2):
                b = 2 + j
                P.op("dve", lambda e, b=b, j=j: e.scalar_tensor_tensor(out=xt[:, b, :], in0=xt[:, b, :], scalar=st_s[:, 4 + j:5 + j],
                                                                       in1=gfb[:, :], op0=ALU.mult, op1=ALU.mult),
                     R=[Rxt[b], Rstat, Rc], W=[Rxt[b]])
                P.dma("pool", y_d[i * 256 + j * 128:i * 256 + (j + 1) * 128, :], xt[:, b, :], R=[Rxt[b]], owner=rs("yout%d" % j))

        def attn(i):
            S2 = idx_scores(i, 2)
            run_interleaved([bisect_gen(S2), chain(kside(i, "rest"), qside("rest"), mixer_a(i, 2), mixer_a(i, 3), gates_gen())])
            bisect_final(S2)
            S3 = idx_scores(i, 3)
            run_interleaved([bisect_gen(S3), mixer_b(i, 2)])
            bisect_final(S3)
            run_interleaved([mixer_b(i, 3)])

        def prefetch(i):
            P.dma("sp", xn, x_d[i * 512:(i + 1) * 512, :].rearrange("(b p) f -> p b f", p=128), W=Rxn + [rs("jk"), rs("jka")],
                  owner=rs("xnload"))
            P.dma("sp", cst[:, :, :], cs_d[:, :, i * 512:(i + 1) * 512].rearrange("t p n -> p t n"), W=[rs("cst")])

        def dump(i):
            for j in range(2):
                b = 2 + j
                P.dma("sp", y_d[i * 256 + j * 128:i * 256 + (j + 1) * 128, :], xt[:, b, :], R=[Rxt[b]], owner=rs("yout%d" % j))

        for i in range(n_tiles):
            state["tile"] = i
            state["wblk"] = 0
            if i == 1 and wscr_d is not None:
                flush_stores()
                for a_, b_ in zip(Rwready, Rwst):
                    a_.w = (b_.sem, b_.semcnt, "dma")
            stages = [
                ("load", lambda: None),
                ("norm1", lambda: rmsnorm_to_hT([0, 1, 2, 3], 0, hTa, rs("hT"), 0, from_xn=True)),
                ("ffn1", lambda: ffn(hTa, rs("hT"), 0, 512, [0, 1, 2, 3], w1a_d, w1b_d, from_xn=True)),
                ("norm2", lambda: rmsnorm_to_hT([0, 1, 2, 3], 1, hTb, rs("hT"), 0)),
                ("early", lambda: run_interleaved([chain(kside(i, "early"), qside("early"))])),
                ("attn", lambda: (attn(i), prefetch(i + 1) if i + 1 < n_tiles else None)),
                ("merge", lambda: merge_and_out()),
                ("norm3", lambda: rmsnorm_to_hT([2, 3], 2, hTa, rs("hT"), 0)),
                ("ffn2", lambda: ffn(hTa, rs("hT"), 0, 256, [2, 3], w2a_d, w2b_d)),
                ("final", lambda: final_out(i)),
            ]
            done = False
            for name, fn in stages:
                fn()
                if name == stop_after:
                    dump(i)
                    done = True
                    break
            if done:
                break
        P.final_wait("pool", [rs("yout0"), rs("yout1")])
        P.emit()
    return nc


SPLIT = (512, 512, 512, 512, 128, 128, 512, 64, 8, 1024, 1024)


def _rope_perm(ncols):
    p = np.arange(ncols)
    d = p % 64
    q = p.copy()
    q[d < 8] = p[d < 8] + 8
    m = (d >= 8) & (d < 16)
    q[m] = p[m] - 8
    return q


def _host_consts():
    c = {}
    c["ident"] = np.eye(128, dtype=np.float32)
    p = np.arange(128)
    em = np.zeros((128, 8, 128), np.float32)
    for g in range(8):
        em[p, g, 16 * g + (p % 16)] = 1.0
    c["emask"] = em.reshape(128, 1024)
    n = np.arange(NIT + 1, dtype=np.float64)
    c["cpw"] = np.broadcast_to((2.0 ** -(n + 1)).astype(np.float32), (128, NIT + 1)).copy()
    k = np.arange(128)[:, None, None]
    rel = np.arange(5)[None, :, None]
    q = np.arange(128)[None, None, :]
    kc_rel = 2 * (rel - 4) + k // 64
    cq = q // 64
    ok = (kc_rel >= cq - 8) & (kc_rel <= cq)
    c["amask"] = np.where(ok, 0.0, NEG).astype(np.float32).reshape(128, 640)
    dist = np.clip(q - k + 512 - 128 * rel, -128, 128) + 128
    c["_dist"] = np.broadcast_to(dist, (128, 5, 128))
    return c


def _rope_tables(origin):
    inv_freq = np.power(np.float32(500000.0), -np.arange(0, 16, 2, dtype=np.float32) / np.float32(16)).astype(np.float32)
    pos = np.maximum(np.arange(SEQ) + origin, 0).astype(np.float32)
    ang = (pos[:, None] * inv_freq[None, :]).astype(np.float32)
    cos = np.cos(ang).astype(np.float32)
    sin = np.sin(ang).astype(np.float32)
    C = np.ones((128, SEQ), np.float32)
    S_ = np.zeros((128, SEQ), np.float32)
    for p in range(128):
        d = p % 64
        if d < 8:
            C[p] = cos[:, d]
            S_[p] = -sin[:, d]
        elif d < 16:
            C[p] = cos[:, d - 8]
            S_[p] = sin[:, d - 8]
    return np.stack([C, S_], 0)


def _prep_shared(inp):
    c = _host_consts()
    w_in = np.asarray(inp["w_in"][0], np.float32)
    offs = np.cumsum((0,) + SPLIT)
    seg = {n: w_in[:, offs[j]:offs[j + 1]] for j, n in enumerate(
        ["qa", "ka", "va", "qb", "kb", "vb", "qi", "ki", "wi", "ga", "gb"])}
    kbp = seg["kb"][:, _rope_perm(128)]
    kip = seg["ki"][:, _rope_perm(64)]
    wk = np.concatenate([seg["ka"], seg["va"], seg["kb"], kbp, seg["vb"], seg["ki"], kip], 1)
    hb_order = np.concatenate([np.r_[j * 64:(j + 1) * 64, (4 + j) * 64:(5 + j) * 64] for j in range(4)])
    qb = seg["qb"][:, hb_order]
    qbp = seg["qb"][:, _rope_perm(512)][:, hb_order]
    qip = seg["qi"][:, _rope_perm(512)]
    wrep = np.repeat(seg["wi"], 16, axis=1)
    pad = np.zeros((D, 384), np.float32)
    wq = np.concatenate([seg["qa"], qb, qbp, seg["qi"], qip, wrep, pad, seg["ga"], seg["gb"]], 1)
    assert wq.shape[1] == 5120 and wk.shape[1] == 1536
    rb = np.asarray(inp["rel_bias"][0], np.float32)
    rbg = rb[:, c["_dist"]]
    rbg = np.ascontiguousarray(np.transpose(rbg, (1, 0, 2, 3))).reshape(128, 8 * 5 * 128)
    gcol = np.stack([np.asarray(inp[k][0], np.float32).reshape(8, 128).T for k in ("n1_g", "n2_g", "n3_g")], 1)
    sh = {
        "w1a": np.ascontiguousarray(inp["ffn1_w_in"][0], np.float32),
        "w1b": np.ascontiguousarray(inp["ffn1_w_out"][0], np.float32),
        "w2a": np.ascontiguousarray(inp["ffn2_w_in"][0], np.float32),
        "w2b": np.ascontiguousarray(inp["ffn2_w_out"][0], np.float32),
        "wk": np.ascontiguousarray(wk), "wq": np.ascontiguousarray(wq),
        "wab": np.ascontiguousarray(np.concatenate([inp["w_branch_a"][0], inp["w_branch_b"][0]], 0), np.float32),
        "wo": np.ascontiguousarray(inp["w_out"][0], np.float32),
        "gcol": np.ascontiguousarray(gcol.reshape(128, 24), np.float32),
        "gf": np.asarray(inp["nf_g"], np.float32).reshape(1, D),
        "rbg": rbg.astype(np.float32), "amask": c["amask"], "emask": c["emask"], "ident": c["ident"], "cpw": c["cpw"],
    }
    return sh


def _prep_core(x, b, hf, sh):
    origin = -256 * (1 - hf)
    xc = np.zeros((SEQ, D), np.float32)
    if hf == 0:
        xc[256:] = x[b, 0:SEQ - 256]
    else:
        xc[:] = x[b]
    kb = np.zeros((128, 32), np.float32)
    kbrow = np.zeros((128, 256), np.float32)
    if hf == 0:
        kb[:, 0:2] = NEG
        kbrow[:] = -200.0
    m = dict(sh)
    m.update({"x": xc, "cs": _rope_tables(origin), "kb": kb, "kbrow": kbrow})
    return m


def kernel(**inputs):
    x = np.asarray(inputs["x"], np.float32)
    sh = _prep_shared(inputs)
    in_maps = [_prep_core(x, c // 2, c % 2, sh) for c in range(8)]
    nc = build(NT)
    res = run_bass_kernel_spmd(nc, in_maps, core_ids=list(range(8)))
    out = np.empty((4, SEQ, D), np.float32)
    for c in range(8):
        b, hf = c // 2, c % 2
        y = np.asarray(res.results[c]["y"], np.float32).reshape(NT, 256, D)
        for i in range(NT):
            p0 = 512 * i + 256 * hf
            out[b, p0:p0 + 256] = y[i]
    return out
```

```python
import contextlib
import numpy as np
import concourse.bass as bass
import concourse.mybir as mybir
from concourse.bass_utils import run_bass_kernel_spmd

F32 = mybir.dt.float32
BF16 = mybir.dt.bfloat16
AF = mybir.ActivationFunctionType
ALU = mybir.AluOpType

D = 1024
DFF = 2816
SEQ = 4096
NT = 8
TILE = 512
NIT = 16
NEG = -30000.0
NS = 5
NBLK = 80


class Res:
    __slots__ = ("name", "w", "r", "sem", "semcnt")

    def __init__(self, name):
        self.name = name
        self.w = None
        self.r = []
        self.sem = None
        self.semcnt = 0


class Prog:
    COMPUTE = ("pe", "act", "dve", "pool")

    def __init__(self, nc, stack):
        self.nc = nc
        self.stack = stack
        self.ops = {e: [] for e in ("pe", "act", "dve", "pool", "sp")}
        self.sems = {}
        self.cnt = {}
        for e in self.COMPUTE:
            self.sems[e] = stack.enter_context(nc.semaphore("s_" + e))
            self.cnt[e] = 0
        self.known = {e: {} for e in self.ops}
        self.ndsem = 0

    def res(self, name):
        return Res(name)

    def _res_sem(self, r):
        if r.sem is None:
            key = "d%d" % self.ndsem
            self.sems[key] = self.stack.enter_context(self.nc.semaphore("sd_%d" % self.ndsem))
            self.ndsem += 1
            r.sem = key
        return r.sem

    def _deps(self, eng, R, W, is_dma):
        waits = {}

        def need(ev, kind):
            if ev is None:
                return
            key, val, e2 = ev
            if (not is_dma) and e2 == eng and key == eng:
                if eng == "pe":
                    return
            if self.known[eng].get(key, 0) >= val:
                return
            if waits.get(key, 0) < val:
                waits[key] = val

        for r in R:
            need(r.w, "raw")
        for r in W:
            need(r.w, "waw")
            for ev in r.r:
                need(ev, "war")
        for k, v in waits.items():
            self.known[eng][k] = v
        return list(waits.items())

    def op(self, eng, fn, R=(), W=(), inc=True):
        waits = self._deps(eng, R, W, False)
        if inc:
            self.cnt[eng] += 1
            ev = (eng, self.cnt[eng], eng)
        else:
            ev = (eng, self.cnt[eng] + 1, eng)
        for r in R:
            r.r.append(ev)
        for r in W:
            r.w = ev
            r.r = []
        self.ops[eng].append((waits, fn, (eng, 1) if inc else None))

    def dma(self, q, out, in_, R=(), W=(), owner=None, **kw):
        if owner is None:
            owner = (list(W) + list(R))[0]
        waits = self._deps(q, R, W, True)
        key = self._res_sem(owner)
        owner.semcnt += 16
        ev = (key, owner.semcnt, "dma")
        for r in R:
            r.r.append(ev)
        for r in W:
            r.w = ev
            r.r = []
        self.ops[q].append((waits, lambda e: e.dma_start(out=out, in_=in_, **kw), (key, 16)))

    def final_wait(self, eng, resources):
        waits = {}
        for r in resources:
            evs = ([r.w] if r.w else []) + list(r.r)
            for key, val, _ in evs:
                if waits.get(key, 0) < val:
                    waits[key] = val
        self.ops[eng].append((list(waits.items()), None, None))

    def emit(self):
        nc = self.nc
        sems = self.sems

        def replay(name):
            def body(e):
                for waits, fn, inc in self.ops[name]:
                    for k, v in waits:
                        e.wait_ge(sems[k], v)
                    if fn is None:
                        continue
                    ins = fn(e)
                    if inc is not None:
                        ins.then_inc(sems[inc[0]], inc[1])
            return body

        with nc.Block() as block:
            block.sync(replay("sp"))
            block.tensor(replay("pe"))
            block.scalar(replay("act"))
            block.vector(replay("dve"))
            block.gpsimd(replay("pool"))


def build(n_tiles=NT, stop_after=None, use_scratch=True):
    nc = bass.Bass("TRN2", target_bir_lowering=False)

    def din(name, shape):
        return nc.dram_tensor(name, list(shape), F32, kind="ExternalInput").ap()

    x_d = din("x", [SEQ, D])
    cs_d = din("cs", [2, 128, SEQ])
    kb_d = din("kb", [128, 32])
    kbrow_d = din("kbrow", [128, 256])
    w1a_d = din("w1a", [D, 2 * DFF])
    w1b_d = din("w1b", [DFF, D])
    w2a_d = din("w2a", [D, 2 * DFF])
    w2b_d = din("w2b", [DFF, D])
    wk_d = din("wk", [D, 1536])
    wq_d = din("wq", [D, 5120])
    wab_d = din("wab", [D, D])
    wo_d = din("wo", [D, D])
    gcol_d = din("gcol", [128, 24])
    gf_d = din("gf", [1, D])
    rbg_d = din("rbg", [128, 8 * 5 * 128])
    amask_d = din("amask", [128, 5 * 128])
    emask_d = din("emask", [128, 8 * 128])
    ident_d = din("ident", [128, 128])
    cpw_d = din("cpw", [128, NIT + 1])
    y_d = nc.dram_tensor("y", [n_tiles * 256, D], F32, kind="ExternalOutput").ap()
    wscr_d = nc.dram_tensor("wscr", [NBLK, 128, 4096], BF16, kind="Internal").ap() if (use_scratch and n_tiles > 1) else None

    with contextlib.ExitStack() as st:
        P = Prog(nc, st)

        def sb(name, shape, dt):
            return st.enter_context(nc.sbuf_tensor(name, list(shape), dt))

        xt = sb("xt", [128, 4, D], F32)
        hTa = sb("hT", [128, 8, TILE], BF16)
        hTb = hTa
        big = sb("big", [128, 12288], BF16)
        aT = big[:, 0:11264].rearrange("p (a b) -> p a b", a=22)
        hb = aT[:, 0:8, :].rearrange("p a b -> p (a b)").rearrange("p (j f) -> p j f", j=4)
        sg = [sb("sg%d" % i, [128, TILE], F32) for i in range(2)]
        kAT = sb("kAT", [128, 4, 1024], BF16)
        vA = sb("vA", [128, 8, 8, 66], BF16)
        kBT = sb("kBT", [128, SEQ], BF16)
        vB = sb("vB", [128, 32, 2, 66], BF16)
        kiT = sb("kiT", [64, SEQ], BF16)
        qAT = [sb("qAT%d" % i, [128, 4, 256], BF16) for i in range(2)]
        qBT = [sb("qBT%d" % i, [128, 4, 256], BF16) for i in range(2)]
        qiT = sb("qiT", [64, 2, 8, 8, 16], BF16)
        wrepT = sb("wrepT", [128, 256], F32)
        score = big[:, 0:8192].bitcast(F32)
        mb = big[:, 8192:12288]
        PT = [sb("PT%d" % i, [128, 8, 128], BF16) for i in range(3)]
        jk = sb("jk", [128, 2 * SEQ], BF16)
        xn = jk[:, :].bitcast(F32).rearrange("p (b f) -> p b f", b=4)
        sqj = PT[0][:, :, :].rearrange("p a b -> p (a b)")
        rt = sg
        cst = sb("cst", [128, 2, TILE], F32)
        biasT = sb("biasT", [128, 8, 5, 128], BF16)
        emask = sb("emask_s", [128, 8, 128], BF16)
        ident = sb("ident_s", [128, 128], BF16)
        gfb = sb("gfb", [128, D], F32)
        gcol = sb("gcol_s", [128, 24], F32)
        kbt = sb("kbt", [128, 32], F32)
        kbrow = sb("kbrow_s", [128, 256], F32)
        cpw = sb("cpw_s", [128, NIT + 1], F32)
        oab = [sb("oab%d" % i, [128, 512], BF16) for i in range(2)]
        oT = sb("oT", [128, 8, 256], BF16)
        mT = sb("mT", [128, 8, 256], BF16)
        wsel = sb("wsel", [128, 8, 128], BF16)
        rl = [sb("rl%d" % i, [128, TILE], BF16) for i in range(2)]
        st_s = sb("stat", [128, 96], F32)
        wsl = [sb("wsl%d" % i, [128, 4096], BF16) for i in range(NS)]
        banks = [st.enter_context(nc.psum_tensor("bk%d" % i, [128, 512], F32)) for i in range(8)]

        R = {}

        def rs(name):
            if name not in R:
                R[name] = P.res(name)
            return R[name]

        Rbank = [P.res("bank%d" % i) for i in range(8)]
        Rwsl = [P.res("wsl%d" % i) for i in range(NS)]
        state = {"bank": 0, "wsl": 0, "alt": 0, "pt": 0, "rl": 0, "sg": 0, "rt": 0, "wblk": 0, "tile": 0}
        Rwst = [P.res("wst%d" % i) for i in range(NS)]
        Rwslh = [P.res("wslh%d" % i) for i in range(NS)]
        Rwready = [P.res("wready%d" % i) for i in range(NS)]

        def bank():
            i = state["bank"]
            state["bank"] = (i + 1) % 6
            return banks[i], Rbank[i]

        def wslot():
            i = state["wsl"]
            state["wsl"] = (i + 1) % NS
            return wsl[i], Rwsl[i]

        def rot(key, n):
            i = state[key]
            state[key] = (i + 1) % n
            return i

        pend_st = []

        def flush_stores():
            for (k_, flat, r, n_el) in pend_st:
                P.dma("sp", wscr_d[k_, :, 0:n_el], flat, R=[r], owner=Rwst[Rwsl.index(r)])
            del pend_st[:]

        def load_w(src2d, kc0, nkc, c0, ncols, slot=None, soff=0):
            if slot is None:
                slot = wslot()
            t, r = slot
            if pend_st and pend_st[0][2] is not r:
                flush_stores()
            n_el = nkc * ncols
            flat = t[:, soff:soff + n_el]
            dst = flat.rearrange("p (k c) -> p k c", k=nkc)
            k_ = state["wblk"]
            state["wblk"] += 1
            assert k_ < NBLK
            if wscr_d is not None and state["tile"] >= 1:
                P.dma("sp", flat, wscr_d[k_, :, 0:n_el], R=Rwready, W=[r], owner=Rwslh[Rwsl.index(r)])
            else:
                src = src2d[kc0 * 128:(kc0 + nkc) * 128, c0:c0 + ncols].rearrange("(k p) c -> p k c", p=128)
                P.dma("pool", dst, src, W=[r])
                if wscr_d is not None:
                    pend_st.append((k_, flat, r, n_el))
            return dst, r

        def load_pair(src2d, c0a, c0b, ncols):
            slot = wslot()
            t, r = slot
            if pend_st and pend_st[0][2] is not r:
                flush_stores()
            n_el = 8 * ncols
            k_ = state["wblk"]
            state["wblk"] += 1
            assert k_ < NBLK
            va = t[:, 0:n_el].rearrange("p (k c) -> p k c", k=8)
            vb = t[:, n_el:2 * n_el].rearrange("p (k c) -> p k c", k=8)
            if wscr_d is not None and state["tile"] >= 1:
                P.dma("sp", t[:, 0:2 * n_el], wscr_d[k_, :, 0:2 * n_el], R=Rwready, W=[r], owner=Rwslh[Rwsl.index(r)])
            else:
                for dst, c0 in ((va, c0a), (vb, c0b)):
                    src = src2d[0:1024, c0:c0 + ncols].rearrange("(k p) c -> p k c", p=128)
                    P.dma("pool", dst, src, W=[r])
                if wscr_d is not None:
                    pend_st.append((k_, t[:, 0:2 * n_el], r, 2 * n_el))
            return va, vb, r

        def mm(out, lhsT, rhs, start, stop, Rr, Ww, last, skip=False):
            P.op("pe", lambda e: e.matmul(out, lhsT, rhs, start=start, stop=stop, skip_group_check=skip), R=Rr, W=Ww, inc=last)

        def evac(out, in_, Rr, Ww, eng=None):
            if eng is None:
                eng = "act" if (state["alt"] % 2 == 0) else "dve"
                state["alt"] += 1
            if eng == "act":
                P.op("act", lambda e: e.activation(out=out, in_=in_, func=AF.Copy), R=Rr, W=Ww)
            else:
                P.op("dve", lambda e: e.tensor_copy(out, in_), R=Rr, W=Ww)

        P.dma("sp", xn, x_d[0:512, :].rearrange("(b p) f -> p b f", p=128),
              W=[rs("xn%d" % b) for b in range(4)] + [rs("jk"), rs("jka")], owner=rs("xnload"))
        P.dma("sp", cst[:, :, :], cs_d[:, :, 0:512].rearrange("t p n -> p t n"), W=[rs("cst")])
        Rc = rs("consts")
        P.dma("sp", gcol[:], gcol_d, W=[Rc])
        P.dma("sp", kbt[:], kb_d, W=[Rc])
        P.dma("sp", kbrow[:], kbrow_d, W=[Rc])
        P.dma("sp", cpw[:], cpw_d, W=[Rc])
        P.dma("sp", gfb[:], bass.AP(gf_d.tensor, 0, [[0, 128], [1, D]]), W=[Rc])
        Rc2 = rs("consts2")
        P.dma("pool", ident[:], ident_d, W=[Rc2])
        P.dma("pool", emask[:].rearrange("p a b -> p (a b)"), emask_d, W=[Rc2])
        Rsc = rs("score")
        Rbias = rs("biasT")
        P.dma("sp", score[:, 2560:3200], amask_d, W=[Rsc])
        for half in range(2):
            P.dma("sp", score[:, 0:2560], rbg_d[:, half * 2560:(half + 1) * 2560], W=[Rsc])
            tv = score[:, 0:2560].rearrange("p (h r) -> p h r", h=4)
            am = score[:, 2560:3200].unsqueeze(1).to_broadcast([128, 4, 640])
            P.op("dve", lambda e, tv=tv, am=am: e.tensor_tensor(out=tv, in0=tv, in1=am, op=ALU.add), R=[Rsc], W=[Rsc])
            bo = biasT[:, half * 4:(half + 1) * 4, :, :].rearrange("p h r q -> p h (r q)")
            P.op("dve", lambda e, tv=tv, bo=bo: e.tensor_scalar(out=bo, in0=tv, scalar1=8.0, scalar2=None, op0=ALU.mult),
                 R=[Rsc], W=[Rbias])
        Rva = [rs("vA%d" % j) for j in range(8)]
        RvB = [rs("vB%d" % j) for j in range(32)]
        for t_ in (qAT[0], qAT[1], qBT[0], qBT[1]):
            P.op("pool", lambda e, t_=t_: e.memset(t_[:, :, :], 0.0), W=[rs("qAT"), rs("qBT")])
        P.op("pool", lambda e: e.memset(vA[:, :, :, 64:66], 1.0), W=Rva)
        P.op("pool", lambda e: e.memset(vB[:, :, :, 64:66], 1.0), W=RvB)

        Rxt = [rs("xt%d" % b) for b in range(4)]
        Rxn = [rs("xn%d" % b) for b in range(4)]
        Rhb = [rs("aT")] * 4
        Rstat = rs("stat")
        RkAT = [rs("kAT%d" % j) for j in range(2)]
        RkB = [rs("kB%d" % j) for j in range(8)]
        Rki = [rs("ki%d" % j) for j in range(8)]

        def rmsnorm_to_hT(blocks, gi, hT, RhT, col0, from_xn=False):
            nb = len(blocks)
            xsrc = xn if from_xn else xt
            Rsrc = (lambda b: [Rxn[b], rs("jk"), rs("jka")]) if from_xn else (lambda b: [Rxt[b]])
            P.op("dve", lambda e: e.memset(st_s[:, 0:8], 0.0), W=[Rstat])
            for j, b in enumerate(blocks):
                P.op("act", lambda e, b=b, j=j: e.activation(out=sqj[:], in_=xsrc[:, b, :], func=AF.Square,
                                                             accum_out=st_s[:, j:j + 1]),
                     R=Rsrc(b) + [Rstat], W=[rs("PT0"), Rstat])
            P.op("dve", lambda e: e.tensor_scalar(out=st_s[:, 4:4 + nb], in0=st_s[:, 0:nb], scalar1=1.0 / D, scalar2=1e-6,
                                                  op0=ALU.mult, op1=ALU.add), R=[Rstat], W=[Rstat])
            P.op("act", lambda e: e.activation(out=st_s[:, 4:4 + nb], in_=st_s[:, 4:4 + nb], func=AF.Sqrt), R=[Rstat], W=[Rstat])
            P.op("dve", lambda e: e.reciprocal(st_s[:, 4:4 + nb], st_s[:, 4:4 + nb]), R=[Rstat], W=[Rstat])
            for j, b in enumerate(blocks):
                P.op("dve", lambda e, b=b, j=j: e.tensor_scalar(out=hb[:, j, :], in0=xsrc[:, b, :], scalar1=st_s[:, 4 + j:5 + j],
                                                               scalar2=None, op0=ALU.mult),
                     R=Rsrc(b) + [Rstat], W=[Rhb[j]])
            for kc in range(8):
                bk, rb = bank()
                for j in range(nb):
                    mm(bk[:, j * 128:(j + 1) * 128], hb[:, j, kc * 128:(kc + 1) * 128], ident[:], True, True,
                       [Rhb[j], Rc2], [rb], j == nb - 1)
                o = hT[:, kc, col0:col0 + nb * 128]
                i_ = bk[:, 0:nb * 128]
                gs = gcol[:, gi * 8 + kc:gi * 8 + kc + 1]
                if kc % 2 == 0:
                    P.op("dve", lambda e, o=o, i_=i_, gs=gs: e.tensor_scalar(out=o, in0=i_, scalar1=gs, scalar2=None, op0=ALU.mult),
                         R=[rb, Rc], W=[RhT])
                else:
                    P.op("act", lambda e, o=o, i_=i_, gs=gs: e.activation(out=o, in_=i_, func=AF.Identity, scale=gs),
                         R=[rb, Rc], W=[RhT])

        def ffn(hT, RhT, col0, ntok, blocks, wa_d, wb_d, from_xn=False):
            RaT = rs("aT")
            for g in range(11):
                wg, wu, rw = load_pair(wa_d, g * 256, DFF + g * 256, 256)
                for jj in range(2):
                    j = g * 2 + jj
                    bg, rbg_ = bank()
                    bu, rbu = bank()
                    for kc in range(8):
                        mm(bg[:, 0:ntok], wg[:, kc, jj * 128:(jj + 1) * 128], hT[:, kc, col0:col0 + ntok], kc == 0, kc == 7,
                           [rw, RhT], [rbg_], kc == 7)
                    for kc in range(8):
                        mm(bu[:, 0:ntok], wu[:, kc, jj * 128:(jj + 1) * 128], hT[:, kc, col0:col0 + ntok], kc == 0, kc == 7,
                           [rw, RhT], [rbu], kc == 7)
                    si = rot("sg", 2)
                    rsg = rs("sg%d" % si)
                    P.op("act", lambda e, si=si, bg=bg: e.activation(out=sg[si][:, 0:ntok], in_=bg[:, 0:ntok], func=AF.Silu),
                         R=[rbg_], W=[rsg])
                    P.op("dve", lambda e, si=si, bu=bu, j=j: e.tensor_tensor(out=aT[:, j, 0:ntok], in0=sg[si][:, 0:ntok],
                                                                             in1=bu[:, 0:ntok], op=ALU.mult),
                         R=[rsg, rbu], W=[RaT])
            for ch in range(2):
                pieces = []
                for (k0, nk) in ((0, 8), (8, 8), (16, 6)):
                    pieces.append(load_w(wb_d, k0, nk, ch * 512, 512))
                for j, b in enumerate(blocks):
                    bk, rb = bank()
                    for kc in range(22):
                        wp, rw = pieces[kc // 8]
                        mm(bk[:, :], aT[:, kc, j * 128:(j + 1) * 128], wp[:, kc % 8, :], kc == 0, kc == 21,
                           [RaT, rw], [rb], kc == 21)
                    xs = xt[:, b, ch * 512:(ch + 1) * 512]
                    xi = xn[:, b, ch * 512:(ch + 1) * 512] if from_xn else xs
                    Ri = [Rxn[b], rs("jk"), rs("jka")] if from_xn else [Rxt[b]]
                    P.op("dve", lambda e, xs=xs, xi=xi, bk=bk: e.scalar_tensor_tensor(out=xs, in0=bk[:, :], scalar=0.5, in1=xi,
                                                                                      op0=ALU.mult, op1=ALU.add),
                         R=[rb] + Ri, W=[Rxt[b]])

        def rope(out, bt, btp, np_, cols, c0, Rr, Ww, psplit=False):
            i1, i2 = 0, 1
            r1, r2 = rs("sg0"), rs("sg1")
            n = 1
            for s_ in bt.shape[1:]:
                n *= s_
            rep = n // cols
            if rep == 1:
                C = cst[0:np_, 0, c0:c0 + cols]
                S_ = cst[0:np_, 1, c0:c0 + cols]
                t1 = rt[i1][0:np_, 0:n]
                t2 = rt[i2][0:np_, 0:n]
            else:
                C = cst[0:np_, 0, c0:c0 + cols].unsqueeze(1).to_broadcast([np_, rep, cols])
                S_ = cst[0:np_, 1, c0:c0 + cols].unsqueeze(1).to_broadcast([np_, rep, cols])
                t1 = rt[i1][0:np_, 0:n].rearrange("p (a b) -> p a b", a=rep)
                t2 = rt[i2][0:np_, 0:n].rearrange("p (a b) -> p a b", a=rep)
            P.op("dve", lambda e: e.tensor_tensor(out=t1, in0=bt, in1=C, op=ALU.mult), R=Rr + [rs("cst")], W=[r1])
            P.op("dve", lambda e: e.tensor_tensor(out=t2, in0=btp, in1=S_, op=ALU.mult), R=Rr + [rs("cst")], W=[r2])
            if psplit:
                for (o_, p0_, p1_) in out:
                    P.op("dve", lambda e, o_=o_, p0_=p0_, p1_=p1_: e.tensor_tensor(out=o_, in0=t1[p0_:p1_], in1=t2[p0_:p1_], op=ALU.add),
                         R=[r1, r2], W=Ww)
            elif isinstance(out, list):
                for (o_, c_, n_, a_) in out:
                    a1 = rt[i1][0:np_, c_:c_ + n_].rearrange("p (a b) -> p a b", a=a_)
                    a2 = rt[i2][0:np_, c_:c_ + n_].rearrange("p (a b) -> p a b", a=a_)
                    P.op("dve", lambda e, o_=o_, a1=a1, a2=a2: e.tensor_tensor(out=o_, in0=a1, in1=a2, op=ALU.add), R=[r1, r2], W=Ww)
            else:
                P.op("dve", lambda e: e.tensor_tensor(out=out, in0=t1, in1=t2, op=ALU.add), R=[r1, r2], W=Ww)

        def kside(i, part):
            RhT = rs("hT")
            slot_j = i % 2
            if part == "early":
                yield from kside_early(i)
                return
            w0, rw0 = load_w(wk_d, 0, 8, 0, 512)
            for c in range(4):
                bk, rb = bank()
                for kc in range(8):
                    mm(bk[:, :], w0[:, kc, c * 128:(c + 1) * 128], hTb[:, kc, :], kc == 0, kc == 7, [rw0, RhT], [rb], kc == 7)
                evac(kAT[:, c, slot_j * 512:(slot_j + 1) * 512], bk[:, :], [rb], [RkAT[slot_j]])
            yield
            w1, rw1 = load_w(wk_d, 0, 8, 512, 512)
            for b in range(4):
                bk, rb = bank()
                for kc in range(8):
                    mm(bk[:, :], hTb[:, kc, b * 128:(b + 1) * 128], w1[:, kc, :], kc == 0, kc == 7, [RhT, rw1], [rb], kc == 7)
                rt_ = (4 * i + b) % 8
                evac(vA[:, rt_, :, 0:64], bk[:, :].rearrange("p (h d) -> p h d", h=8), [rb], [Rva[rt_]])
                if b % 2 == 1:
                    yield

        def kside_early(i):
            RhT = rs("hT")
            w2, rw2 = load_w(wk_d, 0, 8, 1024, 512)
            b1, rb1 = bank()
            b2, rb2 = bank()
            for kc in range(8):
                mm(b1[:, :], w2[:, kc, 0:128], hTb[:, kc, :], kc == 0, kc == 7, [rw2, RhT], [rb1], kc == 7)
            for kc in range(8):
                mm(b2[:, :], w2[:, kc, 128:256], hTb[:, kc, :], kc == 0, kc == 7, [rw2, RhT], [rb2], kc == 7)
            rope(kBT[:, i * 512:(i + 1) * 512], b1[:, :], b2[:, :], 128, 512, 0, [rb1, rb2], [RkB[i]])
            b3, rb3 = bank()
            for b in range(4):
                for kc in range(8):
                    mm(b3[:, b * 128:(b + 1) * 128], hTb[:, kc, b * 128:(b + 1) * 128], w2[:, kc, 256:384], kc == 0, kc == 7,
                       [RhT, rw2], [rb3], (kc == 7 and b == 3))
            evac(vB[:, 4 * i:4 * i + 4, :, 0:64], b3[:, :].rearrange("p (b g d) -> p b g d", b=4, g=2), [rb3],
                 [RvB[4 * i + b] for b in range(4)])
            b4, rb4 = bank()
            b5, rb5 = bank()
            for kc in range(8):
                mm(b4[0:64, :], w2[:, kc, 384:448], hTb[:, kc, :], kc == 0, kc == 7, [rw2, RhT], [rb4], kc == 7)
            for kc in range(8):
                mm(b5[0:64, :], w2[:, kc, 448:512], hTb[:, kc, :], kc == 0, kc == 7, [rw2, RhT], [rb5], kc == 7)
            rope(kiT[0:64, i * 512:(i + 1) * 512], b4[0:64, :], b5[0:64, :], 64, 512, 0, [rb4, rb5], [Rki[i]])
            yield

        def qside(part):
            RhT = rs("hT")
            h2o = lambda kc: hTb[:, kc, 256:512]
            if part == "early":
                yield from qside_early()
                return
            w0, rw0 = load_w(wq_d, 0, 8, 0, 512)
            for c in range(4):
                bk, rb = bank()
                for kc in range(8):
                    mm(bk[:, 0:256], w0[:, kc, c * 128:(c + 1) * 128], h2o(kc), kc == 0, kc == 7, [rw0, RhT], [rb], kc == 7)
                evac(qAT[0][0:64, c, :], bk[0:64, 0:256], [rb], [rs("qAT")])
                evac(qAT[1][64:128, c, :], bk[64:128, 0:256], [rb], [rs("qAT")])
            yield
            w1, rw1 = load_w(wq_d, 0, 8, 512, 512)
            w2, rw2 = load_w(wq_d, 0, 8, 1024, 512)
            for cp in range(2):
                bt, rbt = bank()
                bp, rbp = bank()
                for cc in range(2):
                    c = cp * 2 + cc
                    for kc in range(8):
                        mm(bt[:, cc * 256:(cc + 1) * 256], w1[:, kc, c * 128:(c + 1) * 128], h2o(kc), kc == 0, kc == 7,
                           [rw1, RhT], [rbt], (kc == 7 and cc == 1))
                for cc in range(2):
                    c = cp * 2 + cc
                    for kc in range(8):
                        mm(bp[:, cc * 256:(cc + 1) * 256], w2[:, kc, c * 128:(c + 1) * 128], h2o(kc), kc == 0, kc == 7,
                           [rw2, RhT], [rbp], (kc == 7 and cc == 1))
                outs = [(qBT[0][0:64, cp * 2:cp * 2 + 2, :], 0, 64), (qBT[1][64:128, cp * 2:cp * 2 + 2, :], 64, 128)]
                rope(outs, bt[:, :].rearrange("p (a b) -> p a b", a=2),
                     bp[:, :].rearrange("p (a b) -> p a b", a=2), 128, 256, 256, [rbt, rbp], [rs("qBT")], psplit=True)
                yield

        def qside_early():
            RhT = rs("hT")
            h2o = lambda kc: hTb[:, kc, 256:512]
            w3, rw3 = load_w(wq_d, 0, 8, 1536, 512)
            w4, rw4 = load_w(wq_d, 0, 8, 2048, 512)
            for hp in range(4):
                bt, rbt = bank()
                bp, rbp = bank()
                for hh in range(2):
                    h = hp * 2 + hh
                    for kc in range(8):
                        mm(bt[0:64, hh * 256:(hh + 1) * 256], w3[:, kc, h * 64:(h + 1) * 64], h2o(kc), kc == 0, kc == 7,
                           [rw3, RhT], [rbt], (kc == 7 and hh == 1))
                for hh in range(2):
                    h = hp * 2 + hh
                    for kc in range(8):
                        mm(bp[0:64, hh * 256:(hh + 1) * 256], w4[:, kc, h * 64:(h + 1) * 64], h2o(kc), kc == 0, kc == 7,
                           [rw4, RhT], [rbp], (kc == 7 and hh == 1))
                outs = [(qiT[0:64, bl, :, hp * 2 + hh, :], hh * 256 + bl * 128, 128, 8) for hh in range(2) for bl in range(2)]
                rope(outs, bt[0:64, :].rearrange("p (a b) -> p a b", a=2),
                     bp[0:64, :].rearrange("p (a b) -> p a b", a=2), 64, 256, 256, [rbt, rbp], [rs("qiT")])
            w5, rw5 = load_w(wq_d, 0, 8, 2560, 128)
            bk, rb = bank()
            for kc in range(8):
                mm(bk[:, 0:256], w5[:, kc, :], h2o(kc), kc == 0, kc == 7, [rw5, RhT], [rb], kc == 7)
            evac(wrepT[:, :], bk[:, 0:256], [rb], [rs("wrepT")])
            yield

        def pv_finish(acc_banks, racc, osb, ro, kc0, q0):
            for g in range(2):
                accv = banks[6 + g][:, 0:264].rearrange("p (h d) -> p h d", h=4)
                rec = st_s[:, 16 + 4 * g:20 + 4 * g]
                P.op("dve", lambda e, accv=accv, rec=rec: e.reciprocal(rec, accv[:, :, 64]), R=[racc[g]], W=[Rstat])
                ov = osb[:, g * 256:(g + 1) * 256].rearrange("p (h d) -> p h d", h=4)
                rb_ = rec.unsqueeze(2).to_broadcast([128, 4, 64])
                P.op("dve", lambda e, ov=ov, accv=accv, rb_=rb_: e.tensor_tensor(out=ov, in0=accv[:, :, 0:64], in1=rb_, op=ALU.mult),
                     R=[racc[g], Rstat], W=[ro])
            bk, rb = bank()
            for c in range(4):
                mm(bk[:, c * 128:(c + 1) * 128], osb[:, c * 128:(c + 1) * 128], ident[:], True, True, [ro, Rc2], [rb], c == 3)
            evac(oT[:, kc0:kc0 + 4, q0:q0 + 128], bk[:, :].rearrange("p (c q) -> p c q", c=4), [rb], [rs("oT")])

        Racc = [Rbank[6], Rbank[7]]

        def mixer_a(i, ob):
            q0 = (ob - 2) * 128
            Tq = 4 * i + ob
            tiles = [T for T in range(Tq - 4, Tq + 1) if T >= 0]
            pend = None
            for idx, T in enumerate(tiles):
                rel = T - (Tq - 4)
                col = (T % 8) * 128
                jslot = (T // 4) % 2
                pi = rot("pt", 3)
                rpt = rs("PT%d" % pi)
                for g in range(2):
                    bk, rb = bank()
                    mm(bk[:, :].rearrange("p (h q) -> p h q", h=4), ident[:], biasT[:, g * 4:(g + 1) * 4, rel, :],
                       True, False, [Rc2, Rbias], [rb], False)
                    for hh in range(4):
                        h = g * 4 + hh
                        c, po = h // 2, (h % 2) * 64
                        mm(bk[:, hh * 128:(hh + 1) * 128], kAT[:, c, col:col + 128], qAT[h % 2][:, c, q0:q0 + 128],
                           False, hh == 3, [RkAT[jslot], rs("qAT")], [rb], hh == 3)
                    P.op("act", lambda e, bk=bk, pi=pi, g=g, T=T: e.activation(
                        out=PT[pi][:, g * 4:(g + 1) * 4, :], in_=bk[:, :].rearrange("p (h q) -> p h q", h=4),
                        func=AF.Exp, scale=0.125, bias=kbt[:, T:T + 1]), R=[rb, Rc], W=[rpt])
                if pend is not None:
                    pend()

                def do_pv(T=T, idx=idx, pi=pi, rpt=rpt):
                    for h in range(8):
                        mm(banks[6 + h // 4][:, (h % 4) * 66:(h % 4) * 66 + 66], PT[pi][:, h, :], vA[:, T % 8, h, :],
                           idx == 0 and h % 4 == 0, idx == len(tiles) - 1, [rpt, Rva[T % 8]], [Racc[h // 4]], h % 4 == 3, skip=True)
                pend = do_pv
                yield
            pend()
            pv_finish(None, Racc, oab[0], rs("oab0"), 0, q0)
            yield

        def idx_scores(i, ob):
            q0 = (ob - 2) * 128
            S = 512 * i + 128 * (ob + 1)
            Rw = rs("wsel")
            wb = wrepT[:, q0:q0 + 128].unsqueeze(1).to_broadcast([128, 8, 128])
            P.op("dve", lambda e: e.tensor_tensor(out=wsel[:, :, :], in0=emask[:, :, :], in1=wb, op=ALU.mult),
                 R=[Rc2, rs("wrepT")], W=[Rw])
            cscale = 1.0 / (8.0 * (8.0 ** 0.5))
            for ks in range(0, S, 512):
                n = min(512, S - ks)
                kres = [Rki[ks // 512]]
                ai = 6 + (ks // 512) % 2
                acc, racc = banks[ai], Rbank[ai]
                pend = None
                for g in range(8):
                    bs, rbs = bank()
                    mm(bs[:, 0:n], qiT[0:64, ob - 2, g, :, :].rearrange("p h q -> p (h q)"), kiT[0:64, ks:ks + n], True, True,
                       [rs("qiT")] + kres, [rbs], True)
                    ri = rot("rl", 2)
                    rrl = rs("rl%d" % ri)
                    P.op("act", lambda e, ri=ri, bs=bs, n=n: e.activation(out=rl[ri][:, 0:n], in_=bs[:, 0:n], func=AF.Relu, scale=cscale),
                         R=[rbs], W=[rrl])
                    if pend is not None:
                        pend()

                    def do_acc(g=g, ri=ri, rrl=rrl, n=n):
                        mm(acc[:, 0:n], wsel[:, g, :], rl[ri][:, 0:n], g == 0, g == 7, [Rw, rrl], [racc], g == 7)
                    pend = do_acc
                pend()
                evac(score[:, ks:ks + n], acc[:, 0:n], [racc], [Rsc])
            return S

        def bisect_gen(S):
            Rb = rs("bstat")
            Rcn = rs("bcnt")
            Rsg = rs("bsgn")
            Rjk = rs("jk")
            Rjka = rs("jka")
            split = S >= 1024
            S1 = ((S // 128) // 2) * 128 if split else S
            n2 = S - S1
            P.op("dve", lambda e: e.tensor_tensor(out=score[:, 0:256], in0=score[:, 0:256], in1=kbrow[:, :], op=ALU.add),
                 R=[Rsc, Rc], W=[Rsc])
            P.op("dve", lambda e: e.memset(score[0:64, S - 64:S], -200.0), W=[Rsc])
            sv = score[:, 0:S]
            jv = jk[:, 0:S]
            m8 = st_s[:, 24:32]
            lo0 = st_s[:, 32:33]
            w0 = st_s[:, 33:34]
            cnt = st_s[:, 34:35]
            dd = st_s[:, 35:36]
            mids = [st_s[:, 36:37], st_s[:, 37:38]]
            sgn = st_s[:, 39:40]
            w0c = st_s[:, 40:40 + NIT + 1]
            P.op("dve", lambda e: e.max(out=m8, in_=sv), R=[Rsc], W=[Rb])
            P.op("dve", lambda e: e.tensor_scalar(out=jv, in0=sv, scalar1=-100.0, scalar2=None, op0=ALU.max, op1=ALU.min,
                                                  accum_out=lo0), R=[Rsc, Rb], W=[Rjk, Rjka, Rb])
            P.op("dve", lambda e: e.tensor_tensor(out=w0, in0=m8[:, 0:1], in1=lo0, op=ALU.subtract), R=[Rb], W=[Rb])
            P.op("dve", lambda e: e.tensor_scalar(out=w0, in0=w0, scalar1=1.001, scalar2=1e-4, op0=ALU.mult, op1=ALU.add),
                 R=[Rb], W=[Rb])
            P.op("dve", lambda e: e.tensor_scalar(out=w0c, in0=cpw[:, :], scalar1=w0, scalar2=None, op0=ALU.mult),
                 R=[Rb, Rc], W=[Rb])
            P.op("dve", lambda e: e.tensor_tensor(out=mids[0], in0=lo0, in1=w0c[:, 0:1], op=ALU.add), R=[Rb], W=[Rb])
            yield
            for n in range(NIT):
                mcur, mnext = mids[n % 2], mids[(n + 1) % 2]
                P.op("dve", lambda e, mcur=mcur: e.tensor_scalar(out=jk[:, 0:S1], in0=score[:, 0:S1], scalar1=mcur, scalar2=None,
                                                                 op0=ALU.is_ge, op1=ALU.add, accum_out=cnt),
                     R=[Rsc, Rb], W=[Rjk, Rcn])
                if split:
                    P.op("act", lambda e, mcur=mcur: e.activation(out=jk[:, S1:S], in_=score[:, S1:S], func=AF.Sign, scale=-1.0,
                                                                 bias=mcur, accum_out=sgn), R=[Rsc, Rb], W=[Rjka, Rsg])
                    P.op("dve", lambda e: e.scalar_tensor_tensor(out=cnt, in0=sgn, scalar=-0.5, in1=cnt, op0=ALU.mult, op1=ALU.add),
                         R=[Rsg, Rcn], W=[Rcn])
                tval = 255.5 - n2 / 2.0
                P.op("dve", lambda e, tval=tval: e.tensor_scalar(out=dd, in0=cnt, scalar1=tval, scalar2=0.5, op0=ALU.is_ge,
                                                                 op1=ALU.subtract), R=[Rcn], W=[rs("bdd")])
                P.op("dve", lambda e, mcur=mcur, mnext=mnext, n=n: e.scalar_tensor_tensor(
                    out=mnext, in0=dd, scalar=w0c[:, n:n + 1], in1=mcur, op0=ALU.mult, op1=ALU.add), R=[rs("bdd"), Rb], W=[Rb])
                yield

        def bisect_final(S):
            Rb = rs("bstat")
            thr = st_s[:, 38:39]
            mfin = st_s[:, 36 + NIT % 2:37 + NIT % 2]
            w0c = st_s[:, 40:40 + NIT + 1]
            P.op("dve", lambda e: e.tensor_tensor(out=thr, in0=mfin, in1=w0c[:, NIT:NIT + 1], op=ALU.subtract), R=[Rb], W=[Rb])
            P.op("dve", lambda e: e.tensor_scalar(out=mb[:, 0:S], in0=score[:, 0:S], scalar1=thr, scalar2=8.0 * NEG, op0=ALU.is_lt,
                                                  op1=ALU.mult), R=[Rsc, Rb], W=[rs("mb")])

        def run_interleaved(gens):
            gens = list(gens)
            while gens:
                for g in list(gens):
                    try:
                        next(g)
                    except StopIteration:
                        gens.remove(g)

        def chain(*gs):
            for g in gs:
                yield from g

        def mixer_b(i, ob):
            q0 = (ob - 2) * 128
            S = 512 * i + 128 * (ob + 1)
            nT = S // 128
            Rmb = rs("mb")
            idb = ident[:, :].unsqueeze(1).to_broadcast([128, 4, 128])
            pend = None
            for T in range(nT):
                pi = rot("pt", 3)
                rpt = rs("PT%d" % pi)
                for g in range(2):
                    bk, rb = bank()
                    bv = bk[:, :].rearrange("p (h q) -> p h q", h=4)
                    mm(bv, kBT[:, T * 128:(T + 1) * 128], qBT[g][:, :, q0:q0 + 128], True, False,
                       [RkB[T // 4], rs("qBT")], [rb], False)
                    mm(bv, mb[:, T * 128:(T + 1) * 128], idb, False, True, [Rmb, Rc2], [rb], True)
                    P.op("act", lambda e, bv=bv, pi=pi, g=g: e.activation(out=PT[pi][:, g * 4:(g + 1) * 4, :], in_=bv,
                                                                          func=AF.Exp, scale=0.125), R=[rb], W=[rpt])
                if pend is not None:
                    pend()

                def do_pv(T=T, pi=pi, rpt=rpt):
                    for h in range(8):
                        mm(banks[6 + h // 4][:, (h % 4) * 66:(h % 4) * 66 + 66], PT[pi][:, h, :], vB[:, T, h // 4, :],
                           T == 0 and h % 4 == 0, T == nT - 1, [rpt, RvB[T]], [Racc[h // 4]], h % 4 == 3, skip=True)
                pend = do_pv
                yield
            pend()
            pv_finish(None, Racc, oab[1], rs("oab1"), 4, q0)
            yield

        sgab = xt[:, 0:2, :].rearrange("p a b -> p (a b)").bitcast(BF16).rearrange("p (c t) -> p c t", c=16)

        def gates_gen():
            RhT = rs("hT")
            for half in range(2):
                wga, rwga = load_w(wq_d, 0, 8, 3072 + half * 512, 512)
                wgb, rwgb = load_w(wq_d, 0, 8, 4096 + half * 512, 512)
                for cc in range(4):
                    c = half * 4 + cc
                    bg, rbg_ = bank()
                    for kc in range(8):
                        mm(bg[:, 0:256], wga[:, kc, cc * 128:(cc + 1) * 128], hTb[:, kc, 256:512], kc == 0, kc == 7,
                           [rwga, RhT], [rbg_], False)
                    for kc in range(8):
                        mm(bg[:, 256:512], wgb[:, kc, cc * 128:(cc + 1) * 128], hTb[:, kc, 256:512], kc == 0, kc == 7,
                           [rwgb, RhT], [rbg_], kc == 7)
                    P.op("act", lambda e, c=c, bg=bg: e.activation(out=sgab[:, 2 * c:2 * c + 2, :],
                                                                   in_=bg[:, :].rearrange("p (a t) -> p a t", a=2), func=AF.Sigmoid),
                         R=[rbg_], W=[Rxt[0], Rxt[1]])
                    yield

        def merge_and_out():
            RhT = rs("hT")
            RmT = rs("mT")
            for half in range(2):
                wab, rwab = load_w(wab_d, 0, 8, half * 512, 512)
                for cc in range(4):
                    c = half * 4 + cc
                    bp, rbp = bank()
                    for kc in range(4):
                        mm(bp[:, 0:256], wab[:, kc, cc * 128:(cc + 1) * 128], oT[:, kc, :], kc == 0, kc == 3,
                           [rwab, rs("oT")], [rbp], False)
                    for kc in range(4, 8):
                        mm(bp[:, 256:512], wab[:, kc, cc * 128:(cc + 1) * 128], oT[:, kc, :], kc == 4, kc == 7,
                           [rwab, rs("oT")], [rbp], kc == 7)
                    si = rot("sg", 2)
                    rsg = rs("sg%d" % si)
                    gv = sgab[:, 2 * c:2 * c + 2, :].rearrange("p a t -> p (a t)")
                    P.op("dve", lambda e, si=si, bp=bp, gv=gv: e.tensor_tensor(out=sg[si][:, :], in0=gv, in1=bp[:, :], op=ALU.mult),
                         R=[Rxt[0], Rxt[1], rbp], W=[rsg])
                    P.op("dve", lambda e, si=si, c=c: e.tensor_tensor(out=mT[:, c, :], in0=sg[si][:, 0:256], in1=sg[si][:, 256:512],
                                                                       op=ALU.add), R=[rsg], W=[RmT])
            for ch in range(2):
                wo, rwo = load_w(wo_d, 0, 8, ch * 512, 512)
                for j in range(2):
                    b = 2 + j
                    bk, rb = bank()
                    for kc in range(8):
                        mm(bk[:, :], mT[:, kc, j * 128:(j + 1) * 128], wo[:, kc, :], kc == 0, kc == 7, [RmT, rwo], [rb], kc == 7)
                    xs = xt[:, b, ch * 512:(ch + 1) * 512]
                    P.op("dve", lambda e, xs=xs, bk=bk: e.tensor_tensor(out=xs, in0=bk[:, :], in1=xs, op=ALU.add),
                         R=[rb, Rxt[b]], W=[Rxt[b]])

        def final_out(i):
            P.op("dve", lambda e: e.memset(st_s[:, 0:8], 0.0), W=[Rstat])
            for j in range(2):
                b = 2 + j
                P.op("act", lambda e, b=b, j=j: e.activation(out=sqj[:], in_=xt[:, b, :], func=AF.Square, accum_out=st_s[:, j:j + 1]),
                     R=[Rxt[b], Rstat], W=[rs("PT0"), Rstat])
            P.op("dve", lambda e: e.tensor_scalar(out=st_s[:, 4:6], in0=st_s[:, 0:2], scalar1=1.0 / D, scalar2=1e-6,
                                                  op0=ALU.mult, op1=ALU.add), R=[Rstat], W=[Rstat])
            P.op("act", lambda e: e.activation(out=st_s[:, 4:6], in_=st_s[:, 4:6], func=AF.Sqrt), R=[Rstat], W=[Rstat])
            P.op("dve", lambda e: e.reciprocal(st_s[:, 4:6], st_s[:, 4:6]), R=[Rstat], W=[Rstat])
            for j in range(2):
                b = 2 + j
                P.op("dve", lambda e, b=b, j=j: e.scalar_tensor_tensor(out=xt[:, b, :], in0=xt[:, b, :], scalar=st_s[:, 4 + j:5 + j],
                                                                       in1=gfb[:, :], op0=ALU.mult, op1=ALU.mult),
                     R=[Rxt[b], Rstat, Rc], W=[Rxt[b]])
                P.dma("pool", y_d[i * 256 + j * 128:i * 256 + (j + 1) * 128, :], xt[:, b, :], R=[Rxt[b]], owner=rs("yout%d" % j))

        def attn(i):
            S2 = idx_scores(i, 2)
            run_interleaved([bisect_gen(S2), chain(kside(i, "rest"), qside("rest"), mixer_a(i, 2), mixer_a(i, 3), gates_gen())])
            bisect_final(S2)
            S3 = idx_scores(i, 3)
            run_interleaved([bisect_gen(S3), mixer_b(i, 2)])
            bisect_final(S3)
            run_interleaved([mixer_b(i, 3)])

        def prefetch(i):
            P.dma("sp", xn, x_d[i * 512:(i + 1) * 512, :].rearrange("(b p) f -> p b f", p=128), W=Rxn + [rs("jk"), rs("jka")],
                  owner=rs("xnload"))
            P.dma("sp", cst[:, :, :], cs_d[:, :, i * 512:(i + 1) * 512].rearrange("t p n -> p t n"), W=[rs("cst")])

        def dump(i):
            for j in range(2):
                b = 2 + j
                P.dma("sp", y_d[i * 256 + j * 128:i * 256 + (j + 1) * 128, :], xt[:, b, :], R=[Rxt[b]], owner=rs("yout%d" % j))

        for i in range(n_tiles):
            state["tile"] = i
            state["wblk"] = 0
            if i == 1 and wscr_d is not None:
                flush_stores()
                for a_, b_ in zip(Rwready, Rwst):
                    a_.w = (b_.sem, b_.semcnt, "dma")
            stages = [
                ("load", lambda: None),
                ("norm1", lambda: rmsnorm_to_hT([0, 1, 2, 3], 0, hTa, rs("hT"), 0, from_xn=True)),
                ("ffn1", lambda: ffn(hTa, rs("hT"), 0, 512, [0, 1, 2, 3], w1a_d, w1b_d, from_xn=True)),
                ("norm2", lambda: rmsnorm_to_hT([0, 1, 2, 3], 1, hTb, rs("hT"), 0)),
                ("early", lambda: run_interleaved([chain(kside(i, "early"), qside("early"))])),
                ("attn", lambda: (attn(i), prefetch(i + 1) if i + 1 < n_tiles else None)),
                ("merge", lambda: merge_and_out()),
                ("norm3", lambda: rmsnorm_to_hT([2, 3], 2, hTa, rs("hT"), 0)),
                ("ffn2", lambda: ffn(hTa, rs("hT"), 0, 256, [2, 3], w2a_d, w2b_d)),
                ("final", lambda: final_out(i)),
            ]
            done = False
            for name, fn in stages:
                fn()
                if name == stop_after:
                    dump(i)
                    done = True
                    break
            if done:
                break
        P.final_wait("pool", [rs("yout0"), rs("yout1")])
        P.emit()
    return nc


SPLIT = (512, 512, 512, 512, 128, 128, 512, 64, 8, 1024, 1024)


def _rope_perm(ncols):
    p = np.arange(ncols)
    d = p % 64
    q = p.copy()
    q[d < 8] = p[d < 8] + 8
    m = (d >= 8) & (d < 16)
    q[m] = p[m] - 8
    return q


def _host_consts():
    c = {}
    c["ident"] = np.eye(128, dtype=np.float32)
    p = np.arange(128)
    em = np.zeros((128, 8, 128), np.float32)
    for g in range(8):
        em[p, g, 16 * g + (p % 16)] = 1.0
    c["emask"] = em.reshape(128, 1024)
    n = np.arange(NIT + 1, dtype=np.float64)
    c["cpw"] = np.broadcast_to((2.0 ** -(n + 1)).astype(np.float32), (128, NIT + 1)).copy()
    k = np.arange(128)[:, None, None]
    rel = np.arange(5)[None, :, None]
    q = np.arange(128)[None, None, :]
    kc_rel = 2 * (rel - 4) + k // 64
    cq = q // 64
    ok = (kc_rel >= cq - 8) & (kc_rel <= cq)
    c["amask"] = np.where(ok, 0.0, NEG).astype(np.float32).reshape(128, 640)
    dist = np.clip(q - k + 512 - 128 * rel, -128, 128) + 128
    c["_dist"] = np.broadcast_to(dist, (128, 5, 128))
    return c


def _rope_tables(origin):
    inv_freq = np.power(np.float32(500000.0), -np.arange(0, 16, 2, dtype=np.float32) / np.float32(16)).astype(np.float32)
    pos = np.maximum(np.arange(SEQ) + origin, 0).astype(np.float32)
    ang = (pos[:, None] * inv_freq[None, :]).astype(np.float32)
    cos = np.cos(ang).astype(np.float32)
    sin = np.sin(ang).astype(np.float32)
    C = np.ones((128, SEQ), np.float32)
    S_ = np.zeros((128, SEQ), np.float32)
    for p in range(128):
        d = p % 64
        if d < 8:
            C[p] = cos[:, d]
            S_[p] = -sin[:, d]
        elif d < 16:
            C[p] = cos[:, d - 8]
            S_[p] = sin[:, d - 8]
    return np.stack([C, S_], 0)


def _prep_shared(inp):
    c = _host_consts()
    w_in = np.asarray(inp["w_in"][0], np.float32)
    offs = np.cumsum((0,) + SPLIT)
    seg = {n: w_in[:, offs[j]:offs[j + 1]] for j, n in enumerate(
        ["qa", "ka", "va", "qb", "kb", "vb", "qi", "ki", "wi", "ga", "gb"])}
    kbp = seg["kb"][:, _rope_perm(128)]
    kip = seg["ki"][:, _rope_perm(64)]
    wk = np.concatenate([seg["ka"], seg["va"], seg["kb"], kbp, seg["vb"], seg["ki"], kip], 1)
    hb_order = np.concatenate([np.r_[j * 64:(j + 1) * 64, (4 + j) * 64:(5 + j) * 64] for j in range(4)])
    qb = seg["qb"][:, hb_order]
    qbp = seg["qb"][:, _rope_perm(512)][:, hb_order]
    qip = seg["qi"][:, _rope_perm(512)]
    wrep = np.repeat(seg["wi"], 16, axis=1)
    pad = np.zeros((D, 384), np.float32)
    wq = np.concatenate([seg["qa"], qb, qbp, seg["qi"], qip, wrep, pad, seg["ga"], seg["gb"]], 1)
    assert wq.shape[1] == 5120 and wk.shape[1] == 1536
    rb = np.asarray(inp["rel_bias"][0], np.float32)
    rbg = rb[:, c["_dist"]]
    rbg = np.ascontiguousarray(np.transpose(rbg, (1, 0, 2, 3))).reshape(128, 8 * 5 * 128)
    gcol = np.stack([np.asarray(inp[k][0], np.float32).reshape(8, 128).T for k in ("n1_g", "n2_g", "n3_g")], 1)
    sh = {
        "w1a": np.ascontiguousarray(inp["ffn1_w_in"][0], np.float32),
        "w1b": np.ascontiguousarray(inp["ffn1_w_out"][0], np.float32),
        "w2a": np.ascontiguousarray(inp["ffn2_w_in"][0], np.float32),
        "w2b": np.ascontiguousarray(inp["ffn2_w_out"][0], np.float32),
        "wk": np.ascontiguousarray(wk), "wq": np.ascontiguousarray(wq),
        "wab": np.ascontiguousarray(np.concatenate([inp["w_branch_a"][0], inp["w_branch_b"][0]], 0), np.float32),
        "wo": np.ascontiguousarray(inp["w_out"][0], np.float32),
        "gcol": np.ascontiguousarray(gcol.reshape(128, 24), np.float32),
        "gf": np.asarray(inp["nf_g"], np.float32).reshape(1, D),
        "rbg": rbg.astype(np.float32), "amask": c["amask"], "emask": c["emask"], "ident": c["ident"], "cpw": c["cpw"],
    }
    return sh


def _prep_core(x, b, hf, sh):
    origin = -256 * (1 - hf)
    xc = np.zeros((SEQ, D), np.float32)
    if hf == 0:
        xc[256:] = x[b, 0:SEQ - 256]
    else:
        xc[:] = x[b]
    kb = np.zeros((128, 32), np.float32)
    kbrow = np.zeros((128, 256), np.float32)
    if hf == 0:
        kb[:, 0:2] = NEG
        kbrow[:] = -200.0
    m = dict(sh)
    m.update({"x": xc, "cs": _rope_tables(origin), "kb": kb, "kbrow": kbrow})
    return m


def kernel(**inputs):
    x = np.asarray(inputs["x"], np.float32)
    sh = _prep_shared(inputs)
    in_maps = [_prep_core(x, c // 2, c % 2, sh) for c in range(8)]
    nc = build(NT)
    res = run_bass_kernel_spmd(nc, in_maps, core_ids=list(range(8)))
    out = np.empty((4, SEQ, D), np.float32)
    for c in range(8):
        b, hf = c // 2, c % 2
        y = np.asarray(res.results[c]["y"], np.float32).reshape(NT, 256, D)
        for i in range(NT):
            p0 = 512 * i + 256 * hf
            out[b, p0:p0 + 256] = y[i]
    return out
```
